# Optimizing a Trainium2 kernel written in Bass

```python
import jax, jax.numpy as jnp
from jax import lax
import numpy as np

D_MODEL = 1024
BATCH = 8
SEQ = 4096
DEPTH = 1

PLE_DIM = 256
D_FF = 2816
CONV_CH = D_MODEL
CONV_WIDTH = 31
GLA_HEADS = 4
GLA_DK = D_MODEL // 2
GLA_DV = D_MODEL
GLA_HEAD_K = GLA_DK // GLA_HEADS
GLA_HEAD_V = GLA_DV // GLA_HEADS
GLA_GATE_RANK = 16
GLA_TAU = 16.0
GLA_CHUNK = 64
EPS = 1e-6

kernel_name = "hybrid_conformer_gla_parallel_block"


def rms_norm(x, g):
    x32 = x.astype(jnp.float32)
    y = x32 * lax.rsqrt(jnp.mean(x32 * x32, axis=-1, keepdims=True) + EPS)
    return (y * g.astype(jnp.float32)).astype(x.dtype)


def layer_norm(x, g, b):
    x32 = x.astype(jnp.float32)
    mu = jnp.mean(x32, axis=-1, keepdims=True)
    xc = x32 - mu
    y = xc * lax.rsqrt(jnp.mean(xc * xc, axis=-1, keepdims=True) + EPS)
    return (y * g.astype(jnp.float32) + b.astype(jnp.float32)).astype(x.dtype)


def swiglu(x, w_in, w_out):
    gate, up = jnp.split(x @ w_in, 2, axis=-1)
    return (jax.nn.silu(gate) * up) @ w_out


def conformer_conv(a, b, w_dw, b_dw, ln_g, ln_b, w_pw):
    y = a * jax.nn.sigmoid(b)
    y = lax.conv_general_dilated(
        y, w_dw[:, None, :].astype(y.dtype), window_strides=(1,),
        padding=[(CONV_WIDTH - 1, 0)],
        dimension_numbers=("NWC", "WIO", "NWC"),
        feature_group_count=CONV_CH) + b_dw
    y = jax.nn.silu(layer_norm(y, ln_g, ln_b))
    return y @ w_pw


def gla_chunked(q, k, v, log_a):
    B, T, H, dk = q.shape
    dv = v.shape[-1]
    C = GLA_CHUNK
    N = T // C
    q = q.astype(jnp.float32).reshape(B, N, C, H, dk)
    k = k.astype(jnp.float32).reshape(B, N, C, H, dk)
    v = v.astype(jnp.float32).reshape(B, N, C, H, dv)
    cum = jnp.cumsum(log_a.astype(jnp.float32).reshape(B, N, C, H, dk), axis=2)
    cum_last = cum[:, :, -1:]
    q_dec = q * jnp.exp(cum)
    k_inv = k * jnp.exp(-cum)
    k_dec = k * jnp.exp(cum_last - cum)
    scores = jnp.einsum("bnihd,bnjhd->bnhij", q_dec, k_inv)
    causal = jnp.tril(jnp.ones((C, C), dtype=bool))
    scores = jnp.where(causal, scores, 0.0)
    o_intra = jnp.einsum("bnhij,bnjhe->bnihe", scores, v)
    def step(S, xs):
        qd, kd, vv, a_last = xs
        o = jnp.einsum("bchd,bhde->bche", qd, S)
        S = a_last[..., None] * S + jnp.einsum("bchd,bche->bhde", kd, vv)
        return S, o
    xs = (jnp.moveaxis(q_dec, 1, 0), jnp.moveaxis(k_dec, 1, 0), jnp.moveaxis(v, 1, 0),
          jnp.moveaxis(jnp.exp(cum_last[:, :, 0]), 1, 0))
    S0 = jnp.zeros((B, H, dk, dv), jnp.float32)
    _, o_inter = lax.scan(step, S0, xs)
    o = o_intra + jnp.moveaxis(o_inter, 0, 1)
    return o.reshape(B, T, H, dv)


def setup_inputs(seed: int = 0) -> dict:
    key = jax.random.key(seed)
    ks = jax.random.split(key, 32)
    L, D = DEPTH, D_MODEL
    n_in = 2 * CONV_CH + 2 * GLA_DK + 2 * GLA_DV + GLA_GATE_RANK + 2 * D

    def w(k, shape, fan_in):
        return jax.random.normal(k, shape, jnp.float32) * (fan_in ** -0.5)

    def gain(k, shape):
        return 1.0 + 0.05 * jax.random.normal(k, shape, jnp.float32)

    def bias(k, shape, s=0.02):
        return s * jax.random.normal(k, shape, jnp.float32)

    return {
        "x": jax.random.normal(ks[0], (BATCH, SEQ, D), jnp.float32),
        "p": jax.random.normal(ks[1], (DEPTH, BATCH, SEQ, PLE_DIM), jnp.float32),
        "ffn1_norm": gain(ks[2], (L, D)),
        "ffn1_w_in": w(ks[3], (L, D, 2 * D_FF), D),
        "ffn1_w_out": w(ks[4], (L, D_FF, D), D_FF),
        "mix_norm": gain(ks[5], (L, D)),
        "w_mix_in": w(ks[6], (L, D, n_in), D),
        "conv_dw_w": w(ks[7], (L, CONV_WIDTH, CONV_CH), CONV_WIDTH),
        "conv_dw_b": bias(ks[8], (L, CONV_CH)),
        "conv_ln_g": gain(ks[9], (L, CONV_CH)),
        "conv_ln_b": bias(ks[10], (L, CONV_CH)),
        "conv_w_pw": w(ks[11], (L, CONV_CH, D), CONV_CH),
        "gla_w_alpha": w(ks[12], (L, GLA_GATE_RANK, GLA_DK), GLA_GATE_RANK),
        "gla_b_alpha": bias(ks[13], (L, GLA_DK), 0.1),
        "gla_norm": gain(ks[14], (L, GLA_DV)),
        "gla_w_o": w(ks[15], (L, GLA_DV, D), GLA_DV),
        "w_mix_out": w(ks[16], (L, D, D), D),
        "ffn2_norm": gain(ks[17], (L, D)),
        "ffn2_w_in": w(ks[18], (L, D, 2 * D_FF), D),
        "ffn2_w_out": w(ks[19], (L, D_FF, D), D_FF),
        "ple_norm": gain(ks[20], (L, D)),
        "ple_w_gate": w(ks[21], (L, D, D), D),
        "ple_w_proj": w(ks[22], (L, PLE_DIM, D), PLE_DIM),
        "ple_post_norm": gain(ks[23], (L, D)),
        "final_norm": gain(ks[24], (D,)),
    }


def reference(x, p, ffn1_norm, ffn1_w_in, ffn1_w_out, mix_norm, w_mix_in,
              conv_dw_w, conv_dw_b, conv_ln_g, conv_ln_b, conv_w_pw,
              gla_w_alpha, gla_b_alpha, gla_norm, gla_w_o, w_mix_out,
              ffn2_norm, ffn2_w_in, ffn2_w_out,
              ple_norm, ple_w_gate, ple_w_proj, ple_post_norm, final_norm):
    B, T, D = x.shape
    splits = np.cumsum([CONV_CH, CONV_CH, GLA_DK, GLA_DK, GLA_DV, GLA_DV,
                        GLA_GATE_RANK, D_MODEL]).tolist()
    h = x
    for i in range(DEPTH):
        h = h + 0.5 * swiglu(rms_norm(h, ffn1_norm[i]), ffn1_w_in[i], ffn1_w_out[i])

        u = rms_norm(h, mix_norm[i])
        z = u @ w_mix_in[i]
        (z_ca, z_cb, z_q, z_k, z_v, z_g, z_lr, z_gate_a, z_gate_b) = jnp.split(z, splits, axis=-1)

        y_a = conformer_conv(z_ca, z_cb, conv_dw_w[i], conv_dw_b[i],
                             conv_ln_g[i], conv_ln_b[i], conv_w_pw[i])

        q = z_q.reshape(B, T, GLA_HEADS, GLA_HEAD_K) * (GLA_HEAD_K ** -0.5)
        k = z_k.reshape(B, T, GLA_HEADS, GLA_HEAD_K)
        v = z_v.reshape(B, T, GLA_HEADS, GLA_HEAD_V)
        a_logit = (z_lr @ gla_w_alpha[i] + gla_b_alpha[i]).astype(jnp.float32)
        log_a = (jax.nn.log_sigmoid(a_logit) / GLA_TAU).reshape(B, T, GLA_HEADS, GLA_HEAD_K)
        o = gla_chunked(q, k, v, log_a)
        o = o * lax.rsqrt(jnp.mean(o * o, axis=-1, keepdims=True) + EPS)
        o = o * gla_norm[i].astype(jnp.float32).reshape(GLA_HEADS, GLA_HEAD_V)
        o = o.reshape(B, T, GLA_DV).astype(x.dtype) * jax.nn.silu(z_g)
        y_b = o @ gla_w_o[i]

        merged = jax.nn.sigmoid(z_gate_a) * y_a + jax.nn.sigmoid(z_gate_b) * y_b
        h = h + merged @ w_mix_out[i]

        h = h + 0.5 * swiglu(rms_norm(h, ffn2_norm[i]), ffn2_w_in[i], ffn2_w_out[i])

        gate = jax.nn.sigmoid(rms_norm(h, ple_norm[i]) @ ple_w_gate[i])
        h = h + rms_norm(gate * (p[i].astype(h.dtype) @ ple_w_proj[i]), ple_post_norm[i])
    return rms_norm(h, final_norm)
```

```python
import contextlib
import numpy as np
import concourse.bass as bass
import concourse.mybir as mybir
from concourse.bass_utils import run_bass_kernel_spmd

F32 = mybir.dt.float32
BF16 = mybir.dt.bfloat16
AF = mybir.ActivationFunctionType
ALU = mybir.AluOpType

D = 1024
DFF = 2816
NIN = 7184
PLE = 256
CW = 31
HALO = CW - 1
TT = 512
EPS = 1e-6
NCH = D // 128
NHC = DFF // 128
RING = 5
SLOT = 4096
NPOOL = 48
DEFER_OUT = True
STRICT_SAME_ENGINE = True

ENG_NAMES = ("pe", "act", "dve", "pool", "sp")


class Sched:
    def __init__(self, nc, stack):
        self.nc = nc
        self.stack = stack
        self.prog = {e: [] for e in ENG_NAMES}
        self.count = {e: 0 for e in ENG_NAMES}
        self.known = {e: {} for e in ENG_NAMES}
        self.sems = {}
        self.dma_count = {}
        self.last_write = {}
        self.readers = {}
        for e in ENG_NAMES:
            self.sem(e)

    def sem(self, name):
        if name not in self.sems:
            self.sems[name] = self.stack.enter_context(self.nc.semaphore("s_" + name))
        return self.sems[name]

    def _deps(self, reads, writes, extra):
        deps = set(extra)
        for r in reads:
            if r in self.last_write:
                deps.add(self.last_write[r])
            if isinstance(r, tuple) and r[0] == "ps":
                for ev in self.readers.get(r, ()):
                    deps.add(ev)
        for w in writes:
            if w in self.last_write:
                deps.add(self.last_write[w])
            for ev in self.readers.get(w, ()):
                deps.add(ev)
        return deps

    def _waits(self, eng, deps):
        need = {}
        for (k, v) in deps:
            if k == eng:
                if eng in ("pe", "sp"):
                    continue
                if (not STRICT_SAME_ENGINE) and eng != "pool" and v < self.count[eng]:
                    continue
            if self.known[eng].get(k, 0) >= v:
                continue
            need[k] = max(need.get(k, 0), v)
        for k, v in need.items():
            self.known[eng][k] = v
        return [(self.sems[k], v) for k, v in need.items()]

    def _record(self, ev, reads, writes):
        for w in writes:
            self.last_write[w] = ev
            self.readers[w] = []
        for r in reads:
            if r not in writes:
                self.readers.setdefault(r, []).append(ev)

    def op(self, eng, reads, writes, emit, extra=()):
        reads = list(reads)
        writes = list(writes)
        waits = self._waits(eng, self._deps(reads, writes, extra))
        self.count[eng] += 1
        ev = (eng, self.count[eng])
        sem = self.sems[eng]

        def run(e, waits=waits, emit=emit, sem=sem):
            for s, v in waits:
                e.wait_ge(s, v)
            ins = emit(e)
            ins.then_inc(sem, 1)

        self.prog[eng].append(run)
        self._record(ev, reads, writes)
        return ev

    def dma(self, eng, semname, reads, writes, emit, extra=()):
        reads = list(reads)
        writes = list(writes)
        self.sem(semname)
        waits = self._waits(eng, self._deps(reads, writes, extra))
        self.dma_count[semname] = self.dma_count.get(semname, 0) + 1
        ev = (semname, 16 * self.dma_count[semname])
        sem = self.sems[semname]

        def run(e, waits=waits, emit=emit, sem=sem):
            for s, v in waits:
                e.wait_ge(s, v)
            ins = emit(e)
            ins.then_inc(sem, 16)

        self.prog[eng].append(run)
        self._record(ev, reads, writes)
        return ev

    def final_wait(self, eng, events):
        waits = self._waits(eng, set(events))

        def run(e, waits=waits):
            for s, v in waits:
                e.wait_ge(s, v)

        self.prog[eng].append(run)

    def emit_all(self, block):
        progs = self.prog

        @block.tensor
        def _(e):
            for f in progs["pe"]:
                f(e)

        @block.scalar
        def _(e):
            for f in progs["act"]:
                f(e)

        @block.vector
        def _(e):
            for f in progs["dve"]:
                f(e)

        @block.gpsimd
        def _(e):
            for f in progs["pool"]:
                f(e)

        @block.sync
        def _(e):
            for f in progs["sp"]:
                f(e)


MIX_SLABS = {
    "ca0": (0, 512), "ca1": (512, 512), "cb0": (1024, 512), "cb1": (1536, 512),
    "q": (2048, 512), "k": (2560, 512), "v0": (3072, 512), "v1": (3584, 512),
    "g0": (4096, 512), "g1": (4608, 512), "lr": (5120, 16),
    "ga0": (5136, 512), "ga1": (5648, 512), "gb0": (6160, 512), "gb1": (6672, 512),
}
MIX_ORDER = list(MIX_SLABS.keys())

WEIGHT_NAMES = ["ffn1_w_in", "ffn1_w_out", "w_mix_in", "conv_w_pw", "gla_w_o", "w_mix_out",
                "ffn2_w_in", "ffn2_w_out", "ple_w_gate", "ple_w_proj"]


def slab_table():
    tab = {}
    for nm in ("ffn1_w_in", "ffn2_w_in"):
        sl = []
        for j in range(11):
            pieces = []
            for g in range(2):
                pieces.append((g * 256, 0, 8, g * DFF + j * 256, 256, 512))
            sl.append((8 * 512, pieces))
        tab[nm] = sl
    for nm in ("ffn1_w_out", "ffn2_w_out"):
        sl = []
        for s in range(8):
            cg, half = s // 2, s % 2
            sl.append((11 * 256, [(0, half * 11, 11, cg * 256, 256, 256)]))
        tab[nm] = sl
    sl = []
    for nm in MIX_ORDER:
        c0, ncol = MIX_SLABS[nm]
        sl.append((8 * ncol, [(0, 0, 8, c0, ncol, ncol)]))
    tab["w_mix_in"] = sl
    for nm in ("conv_w_pw", "gla_w_o", "w_mix_out", "ple_w_gate"):
        tab[nm] = [(8 * 512, [(0, 0, 8, s * 512, 512, 512)]) for s in range(2)]
    tab["ple_w_proj"] = [(2 * 1024, [(0, 0, 2, 0, 1024, 1024)])]
    return tab


def build_program(T, dbg=False):
    NT = T // TT
    nc = bass.Bass("TRN2", target_bir_lowering=False)
    tab = slab_table()

    def din(name, shape):
        return nc.dram_tensor(name, list(shape), F32, kind="ExternalInput").ap()

    x_d = din("x", (T, D))
    p_d = din("p", (T, PLE))
    W = {}
    W["ffn1_w_in"] = din("ffn1_w_in", (D, 2 * DFF))
    W["ffn1_w_out"] = din("ffn1_w_out", (DFF, D))
    W["w_mix_in"] = din("w_mix_in", (D, NIN))
    W["conv_w_pw"] = din("conv_w_pw", (D, D))
    W["gla_w_o"] = din("gla_w_o", (D, D))
    W["w_mix_out"] = din("w_mix_out", (D, D))
    W["ffn2_w_in"] = din("ffn2_w_in", (D, 2 * DFF))
    W["ffn2_w_out"] = din("ffn2_w_out", (DFF, D))
    W["ple_w_gate"] = din("ple_w_gate", (D, D))
    W["ple_w_proj"] = din("ple_w_proj", (PLE, D))
    VEC_NAMES = ["ffn1_norm", "mix_norm", "conv_dw_b", "conv_ln_g", "conv_ln_b", "gla_norm",
                 "ffn2_norm", "ple_norm", "ple_post_norm", "final_norm"]
    V = {n: din(n, (1, D)) for n in VEC_NAMES}
    dww_d = din("conv_dw_w", (CW, D))
    walpha_d = din("gla_w_alpha", (16, 512))
    balpha_d = din("gla_b_alpha", (1, 512))
    y_d = nc.dram_tensor("y", [T, D], F32, kind="ExternalOutput").ap()
    dbg_d = None
    if dbg:
        dbg_d = nc.dram_tensor("dbg", [8, 128, NCH, TT], F32, kind="ExternalOutput").ap()

    scr = {}
    for nm in WEIGHT_NAMES:
        nsl = len(tab[nm])
        scr[nm] = nc.dram_tensor("scr_" + nm, [nsl, 128, SLOT], BF16, kind="Internal").ap()

    with contextlib.ExitStack() as st:
        def sb(name, shape, dt):
            return st.enter_context(nc.sbuf_tensor(name, list(shape), dt))

        hh = sb("hh", (128, 2 * NCH, TT), F32)
        xin = sb("xin", (128, 4, D), F32)
        pin = sb("pin", (128, 4, PLE), F32)
        ft = sb("ft", (128, NCH, TT), F32)
        pool = sb("pool", (128, NPOOL, TT), BF16)
        ycv = sb("ycv", (128, NCH, HALO + TT), BF16)
        vtok = sb("vtok", (128, 4, D), BF16)
        kdtok = sb("kdtok", (128, 4, 512), BF16)
        ring = sb("ring", (128, RING, SLOT), BF16)
        rowb = sb("rowb", (128, 4, TT), F32)
        gl4 = sb("gl4", (128, TT), F32)
        Sst = sb("Sst", (128, 4, 256), F32)
        Sb = sb("Sb", (128, 8, 256), BF16)
        scm = sb("scm", (128, 4, 128), BF16)
        zerob = sb("zerob", (128, 128), BF16)
        identf = sb("identf", (128, 128), F32)
        onesf = sb("onesf", (128, 128), F32)
        identb = sb("identb", (128, 128), BF16)
        ones_s = sb("ones_s", (128, 128), BF16)
        maskT = sb("maskT", (128, 128), F32)
        triU = sb("triU", (128, 128), F32)
        rmask = sb("rmask", (128, TT), F32)
        vecT = sb("vecT", (128, NCH, 64), F32)
        wal = sb("wal", (32, 512), BF16)
        lrT = sb("lrT", (32, TT), BF16)
        pT = sb("pT", (128, 2, TT), BF16)
        alast = sb("alast", (128, 4, 8), F32)
        ps = [st.enter_context(nc.psum_tensor("ps%d" % i, [128, 512], F32)) for i in range(8)]

        S = Sched(nc, st)
        block = st.enter_context(nc.Block())

        mm_banks = [0, 1, 4, 5, 6, 7]
        mm_ctr = [0]

        def mmbank():
            b = mm_banks[mm_ctr[0] % len(mm_banks)]
            mm_ctr[0] += 1
            return b

        ST_BANK = 2
        SC_BANK = 3

        def PS(b):
            return ("ps", b)

        def PL(i):
            return ("pool", i)

        HP = [0]

        def Hk(i):
            return ("h", HP[0], i)

        def hv(c):
            return hh[:, HP[0] * NCH + c, :]

        def FT(i):
            return ("ft", i)

        def ALLH_():
            return [Hk(i) for i in range(NCH)]
        ALLFT = [FT(i) for i in range(NCH)]

        U0, SQ0, X0 = 0, 8, 16
        HID0 = 16
        YACT0, MRG0 = 16, 24
        DG0 = 32
        QD0, KI0, GS0, OG0 = 32, 36, 16, 40

        VC = {n: 32 + i for i, n in enumerate(VEC_NAMES)}

        def vcol(name, c):
            return vecT[:, c, VC[name]:VC[name] + 1]

        S.op("pool", [], ["identf"], lambda e: e.memset(identf[:], 0.0))
        S.op("pool", ["identf"], ["identf"], lambda e: e.affine_select(
            out=identf[:], in_=identf[:], pattern=[[-1, 128]], compare_op=ALU.not_equal, fill=1.0,
            base=0, channel_multiplier=1))
        S.op("pool", [], ["onesf"], lambda e: e.memset(onesf[:], 1.0))
        S.op("pool", [], ["zerob"], lambda e: e.memset(zerob[:], 0.0))
        S.op("pool", ["identf"], ["identb"], lambda e: e.tensor_copy(out=identb[:], in_=identf[:]))
        S.op("pool", [], ["ones_s"], lambda e: e.memset(ones_s[:], 1.0 / 1024.0))

        S.op("pool", ["onesf"], ["maskT"], lambda e: e.affine_select(
            out=maskT[:], in_=onesf[:], pattern=[[1, 128]], compare_op=ALU.is_ge, fill=0.0, base=0, channel_multiplier=-1))
        S.op("pool", ["onesf"], ["triU"], lambda e: e.affine_select(
            out=triU[:], in_=onesf[:], pattern=[[-1, 128]], compare_op=ALU.is_gt, fill=0.0, base=0, channel_multiplier=1))

        S.op("pool", [], ["rmask"], lambda e: e.memset(rmask[:], 1.0))
        S.op("pool", ["rmask"], ["rmask"], lambda e: e.memset(rmask[:].rearrange("p (c k) -> p c k", k=128)[:, :, 0:1], 0.0))
        S.op("pool", [], ["lrT"], lambda e: e.memset(lrT[:], 1.0))
        S.op("pool", [], ["Sst"], lambda e: e.memset(Sst[:], 0.0))
        S.op("pool", [], [("Sb", i) for i in range(8)], lambda e: e.memset(Sb[:], 0.0))
        S.op("pool", [], [("ycvh", c) for c in range(NCH)], lambda e: e.memset(ycv[:, :, 0:HALO], 0.0))
        xin_zero_ev = S.op("pool", [], ["xin"], lambda e: e.memset(xin[:, 0, :], 0.0))

        def ld_vecs(e):
            ins = e.dma_start(out=xin[0:CW, 0, :], in_=dww_d)
            return ins

        S.dma("pool", "cst_dw", [], ["xin_dw"], ld_vecs, extra=[xin_zero_ev])
        for i, n in enumerate(VEC_NAMES):
            S.dma("pool", "cst_v%d" % i, [], ["xin_v%d" % i],
                  (lambda e, i=i, n=n: e.dma_start(out=xin[32 + i:33 + i, 0, :], in_=V[n])), extra=[xin_zero_ev])
        S.dma("pool", "cst_wa", [], ["wal_a"], lambda e: e.dma_start(out=wal[0:16, :], in_=walpha_d))
        S.dma("pool", "cst_wb", [], ["wal_b"], lambda e: e.dma_start(out=wal[16:17, :], in_=balpha_d))

        for c in range(NCH):
            b = mmbank()
            S.op("pe", ["xin", "xin_dw", "identf"] + ["xin_v%d" % i for i in range(len(VEC_NAMES))], [PS(b)], lambda e, c=c, b=b: e.transpose(
                out=ps[b][:, 0:128], in_=xin[:, 0, c * 128:(c + 1) * 128], identity=identf[:]))
            S.op("dve", [PS(b)], [("vecT", c)], lambda e, c=c, b=b: e.tensor_copy(
                out=vecT[:, c, :], in_=ps[b][:, 0:64]))
            S.op("dve", [("vecT", c)], [("vecT", c)], lambda e, c=c: e.tensor_scalar(
                out=vecT[:, c, 63:64], in0=vecT[:, c, VC["ple_post_norm"]:VC["ple_post_norm"] + 1], scalar1=0.5, scalar2=None,
                op0=ALU.mult))
        VECS = [("vecT", c) for c in range(NCH)]

        stream = []
        for t in range(NT):
            for nm in ("ffn1_w_in", "ffn1_w_out"):
                for si in range(len(tab[nm])):
                    stream.append((nm, si))
            mo = {n: i for i, n in enumerate(MIX_ORDER)}
            for key in ("ca0", "cb0", "ca1", "cb1", "v0", "v1"):
                stream.append(("w_mix_in", mo[key]))
            stream += [("w_mix_in", mo["ga0"]), ("conv_w_pw", 0), ("w_mix_in", mo["ga1"]), ("conv_w_pw", 1)]
            for key in ("lr", "q", "k", "g0", "g1"):
                stream.append(("w_mix_in", mo[key]))
            stream += [("w_mix_in", mo["gb0"]), ("w_mix_in", mo["gb1"]), ("gla_w_o", 0), ("gla_w_o", 1)]
            stream += [("w_mix_out", 0), ("w_mix_out", 1)]
            for nm in ("ffn2_w_in", "ffn2_w_out"):
                for si in range(len(tab[nm])):
                    stream.append((nm, si))
            stream += [("ple_w_proj", 0), ("ple_w_gate", 0), ("ple_w_gate", 1)]
        st_loaded = [0]
        st_used = [0]

        def pump():
            while st_loaded[0] < len(stream) and st_loaded[0] < st_used[0] + RING:
                i = st_loaded[0]
                nm, si = stream[i]
                slot = i % RING
                elems = tab[nm][si][0]
                S.dma("sp", "ring%d" % slot, [], [("ring", slot)],
                      (lambda e, nm=nm, si=si, slot=slot, elems=elems: e.dma_start(
                          out=ring[:, slot, 0:elems], in_=scr[nm][si, :, 0:elems])),
                      extra=[cast_ev.get((nm, si), cast_ev[nm])])
                st_loaded[0] += 1

        def next_slab(nm_expect, si_expect):
            i = st_used[0]
            assert stream[i] == (nm_expect, si_expect), (stream[i], nm_expect, si_expect)
            st_used[0] += 1
            return i % RING

        def release():
            pump()

        def rms_stats(src_regions, src_ap_fn, nchunks, scale, rstd_slot, sq_base=SQ0):
            for c in range(nchunks):
                S.op("act", [src_regions[c]], [PL(sq_base + c)], lambda e, c=c: e.activation(
                    out=pool[:, sq_base + c, :], in_=src_ap_fn(c), func=AF.Square))
            S.op("pe", [PL(sq_base + c) for c in range(nchunks)] + ["ones_s"], [PS(ST_BANK)],
                 lambda e: [e.matmul(out=ps[ST_BANK][:], lhsT=ones_s[:], rhs=pool[:, sq_base + c, :],
                                     start=(c == 0), stop=(c == nchunks - 1)) for c in range(nchunks)][-1])
            S.op("act", [PS(ST_BANK)], [("rowb", rstd_slot)], lambda e: e.activation(
                out=rowb[:, rstd_slot, :], in_=ps[ST_BANK][:], func=AF.Sqrt, scale=scale, bias=EPS))
            S.op("dve", [("rowb", rstd_slot)], [("rowb", rstd_slot)], lambda e: e.reciprocal(
                out=rowb[:, rstd_slot, :], in_=rowb[:, rstd_slot, :]))

        hstat = {"pending": None, "n": 0}

        def _hstat_mm(m):
            n = hstat["n"]
            S.op("pe", [PL(SQ0 + m), "ones_s"], [PS(ST_BANK)], lambda e, m=m, n=n: e.matmul(
                out=ps[ST_BANK][:], lhsT=ones_s[:], rhs=pool[:, SQ0 + m, :], start=(n == 0), stop=(n == NCH - 1)))
            hstat["n"] = n + 1

        def h_updated(m):
            S.op("act", [Hk(m)], [PL(SQ0 + m)], lambda e, m=m, hm=hv(m): e.activation(
                out=pool[:, SQ0 + m, :], in_=hm, func=AF.Square))
            if hstat["pending"] is not None:
                _hstat_mm(hstat["pending"])
            hstat["pending"] = m

        def h_stats_finish(rstd_slot):
            _hstat_mm(hstat["pending"])
            assert hstat["n"] == NCH
            hstat["pending"] = None
            hstat["n"] = 0
            S.op("act", [PS(ST_BANK)], [("rowb", rstd_slot)], lambda e: e.activation(
                out=rowb[:, rstd_slot, :], in_=ps[ST_BANK][:], func=AF.Sqrt, scale=1.0, bias=EPS))
            S.op("dve", [("rowb", rstd_slot)], [("rowb", rstd_slot)], lambda e: e.reciprocal(
                out=rowb[:, rstd_slot, :], in_=rowb[:, rstd_slot, :]))

        def rmsnorm_h_to_u(gname):
            h_stats_finish(0)
            for c in range(NCH):
                S.op("dve", [Hk(c), ("rowb", 0)] + VECS, [PL(U0 + c)], lambda e, c=c, hm=hv(c): e.scalar_tensor_tensor(
                    out=pool[:, U0 + c, :], in0=hm, scalar=vcol(gname, c), in1=rowb[:, 0, :],
                    op0=ALU.mult, op1=ALU.mult))

        def proj_fm(slot, col_off, kcs, rhs_fn, rhs_regions, bank, kstride, stream_first=False):
            if stream_first:
                kl = list(kcs)
                for i, kc in enumerate(kl):
                    S.op("pe", [("ring", slot), rhs_regions[i]], [PS(bank)], lambda e, i=i, kc=kc: e.matmul(
                        out=ps[bank][:], lhsT=ring[:, slot, kc * kstride + col_off: kc * kstride + col_off + 128],
                        rhs=rhs_fn(kc), start=(i == 0), stop=(i == len(kl) - 1)))
                return

            def emit(e):
                ins = None
                for i, kc in enumerate(kcs):
                    ins = e.matmul(out=ps[bank][:], lhsT=ring[:, slot, kc * kstride + col_off: kc * kstride + col_off + 128],
                                   rhs=rhs_fn(kc), start=(i == 0), stop=(i == len(kcs) - 1))
                return ins
            S.op("pe", [("ring", slot)] + rhs_regions, [PS(bank)], emit)

        UREG = [PL(U0 + c) for c in range(NCH)]

        def ffn(win, wout, gname, after_norm=None):
            rmsnorm_h_to_u(gname)
            if after_norm is not None:
                after_norm()
            for j in range(11):
                slot = next_slab(win, j)
                for s in range(2):
                    hc = 2 * j + s
                    bg, bu = mmbank(), mmbank()
                    proj_fm(slot, s * 128, range(8), lambda kc: pool[:, U0 + kc, :], UREG, bg, 512, stream_first=(hc == 0))
                    proj_fm(slot, 256 + s * 128, range(8), lambda kc: pool[:, U0 + kc, :], UREG, bu, 512)
                    S.op("act", [PS(bg)], [PL(SQ0 + (hc % 8))], lambda e, bg=bg, hc=hc: e.activation(
                        out=pool[:, SQ0 + (hc % 8), :], in_=ps[bg][:], func=AF.Silu))
                    S.op("dve", [PS(bu), PL(SQ0 + (hc % 8))], [PL(HID0 + hc)], lambda e, bu=bu, hc=hc: e.tensor_tensor(
                        out=pool[:, HID0 + hc, :], in0=ps[bu][:], in1=pool[:, SQ0 + (hc % 8), :], op=ALU.mult))
                release()
            for cg in range(4):
                s0 = next_slab(wout, 2 * cg)
                s1 = next_slab(wout, 2 * cg + 1)
                for s in range(2):
                    m = 2 * cg + s
                    b = mmbank()

                    def emit(e, s0=s0, s1=s1, s=s, b=b):
                        ins = None
                        for half, slot in ((0, s0), (1, s1)):
                            for kc in range(11):
                                hc = half * 11 + kc
                                ins = e.matmul(out=ps[b][:], lhsT=ring[:, slot, kc * 256 + s * 128: kc * 256 + s * 128 + 128],
                                               rhs=pool[:, HID0 + hc, :], start=(hc == 0), stop=(hc == 21))
                        return ins
                    S.op("pe", [("ring", s0), ("ring", s1)] + [PL(HID0 + i) for i in range(NHC)], [PS(b)], emit)
                    S.op("dve", [PS(b), Hk(m)], [Hk(m)], lambda e, b=b, m=m, hm=hv(m): e.scalar_tensor_tensor(
                        out=hm, in0=ps[b][:], scalar=0.5, in1=hm, op0=ALU.mult, op1=ALU.add))
                    h_updated(m)
                release()

        def dump(k):
            if dbg:
                S.dma("pool", "dbg", ALLH_(), [], lambda e, k=k, hp=HP[0]: e.dma_start(out=dbg_d[k], in_=hh[:, hp * NCH:(hp + 1) * NCH, :]))

        out_evs = []
        x_t = x_d.rearrange("(n blk p) f -> n p blk f", p=128, blk=4)
        p_t = p_d.rearrange("(n blk p) f -> n p blk f", p=128, blk=4)
        y_t = y_d.rearrange("(n blk p) f -> n p blk f", p=128, blk=4)

        def load_x(t):
            S.dma("sp", "xin", [], ["xin"], lambda e, t=t: e.dma_start(out=xin[:], in_=x_t[t]))
            S.dma("sp", "pin", [], ["pin"], lambda e, t=t: e.dma_start(out=pin[:], in_=p_t[t]))

        load_x(0)
        pending_out = []
        emit_out_ref = [None]
        cast_ev = {}
        for nm in WEIGHT_NAMES:
            wv = W[nm].rearrange("(kc p) n -> p kc n", p=128)
            ev = None
            gate = []
            wi = WEIGHT_NAMES.index(nm)
            if False and wi >= 2:
                gate = [cast_ev[WEIGHT_NAMES[wi - 1]]]
            for si, (elems, pieces) in enumerate(tab[nm]):
                fine = nm in ("ffn1_w_in", "ffn1_w_out")
                for (doff, kc0, nkc, col0, ncols, dstride) in pieces:
                    src = wv[:, kc0:kc0 + nkc, col0:col0 + ncols]
                    dst = scr[nm][si, :, 0:nkc * dstride].rearrange("p (kc n) -> p kc n", n=dstride)[:, :, doff:doff + ncols]
                    semn = ("cast_%s_%d" % (nm, si)) if fine else ("cast_" + nm)
                    ev = S.dma("pool", semn, [], [], (lambda e, src=src, dst=dst: e.dma_start(out=dst, in_=src)), extra=gate)
                    gate = []
                if fine:
                    cast_ev[(nm, si)] = ev
            cast_ev[nm] = ev

        pump()

        def emit_out(t, blks=(0, 1, 2, 3), store=True):
            hp = t % 2
            hst = hh[:, hp * NCH:(hp + 1) * NCH, :].rearrange("p c t -> p (c t)").rearrange("p (blk f) -> p blk f", blk=4)
            HR = [("h", hp, i) for i in range(NCH)]
            for blk in blks:
                for fh in range(2):
                    b = mmbank()
                    S.op("pe", ALLFT + ["identf"], [PS(b)], lambda e, blk=blk, fh=fh, b=b: [e.transpose(
                        out=ps[b][:, j * 128:(j + 1) * 128], in_=ft[:, fh * 4 + j, blk * 128:(blk + 1) * 128],
                        identity=identf[:]) for j in range(4)][-1])
                    if fh:
                        S.op("act", [PS(b)], HR, lambda e, blk=blk, fh=fh, b=b, hst=hst: e.activation(
                            out=hst[:, blk, fh * 512:(fh + 1) * 512], in_=ps[b][:], func=AF.Copy))
                    else:
                        S.op("dve", [PS(b)], HR, lambda e, blk=blk, fh=fh, b=b, hst=hst: e.tensor_copy(
                            out=hst[:, blk, fh * 512:(fh + 1) * 512], in_=ps[b][:]))
            if store:
                ev = S.dma("sp", "yout%d" % hp, HR, [], lambda e, t=t, hst=hst: e.dma_start(out=y_t[t], in_=hst))
                out_evs.append(ev)

        emit_out_ref[0] = emit_out

        for t in range(NT):
            HP[0] = t % 2
            for fc in range(NCH):
                b = mmbank()
                S.op("pe", ["xin", "identf"], [PS(b)], lambda e, fc=fc, b=b: [e.transpose(
                    out=ps[b][:, blk * 128:(blk + 1) * 128], in_=xin[:, blk, fc * 128:(fc + 1) * 128],
                    identity=identf[:]) for blk in range(4)][-1])
                eng = "act" if fc % 2 else "dve"
                if eng == "act":
                    S.op("act", [PS(b)], [Hk(fc)], lambda e, fc=fc, b=b, hm=hv(fc): e.activation(out=hm, in_=ps[b][:], func=AF.Copy))
                else:
                    S.op("dve", [PS(b)], [Hk(fc)], lambda e, fc=fc, b=b, hm=hv(fc): e.tensor_copy(out=hm, in_=ps[b][:]))
                h_updated(fc)
            for pc in range(2):
                b = mmbank()
                S.op("pe", ["pin", "identf"], [PS(b)], lambda e, pc=pc, b=b: [e.transpose(
                    out=ps[b][:, blk * 128:(blk + 1) * 128], in_=pin[:, blk, pc * 128:(pc + 1) * 128],
                    identity=identf[:]) for blk in range(4)][-1])
                S.op("dve", [PS(b)], [("pT", pc)], lambda e, pc=pc, b=b: e.tensor_copy(out=pT[:, pc, :], in_=ps[b][:]))
            if t > 0 and DEFER_OUT:
                emit_out_ref[0](t - 1, blks=(0, 1), store=False)
            if t + 1 < NT:
                load_x(t + 1)
            if t == 0:
                dump(0)

            ffn("ffn1_w_in", "ffn1_w_out", "ffn1_norm",
                after_norm=(lambda t=t: emit_out_ref[0](t - 1, blks=(2, 3), store=True)) if (t > 0 and DEFER_OUT) else None)
            if t == 0:
                dump(1)

            rmsnorm_h_to_u("mix_norm")
            for half in range(2):
                sa = next_slab("w_mix_in", MIX_ORDER.index("ca%d" % half))
                sbb = next_slab("w_mix_in", MIX_ORDER.index("cb%d" % half))
                for s in range(4):
                    cc = half * 4 + s
                    ba, bb_ = mmbank(), mmbank()
                    proj_fm(sa, s * 128, range(8), lambda kc: pool[:, U0 + kc, :], UREG, ba, 512, stream_first=(cc == 0))
                    proj_fm(sbb, s * 128, range(8), lambda kc: pool[:, U0 + kc, :], UREG, bb_, 512)
                    S.op("act", [PS(bb_)], [("rowb", 2 + (cc % 2))], lambda e, bb_=bb_, cc=cc: e.activation(
                        out=rowb[:, 2 + (cc % 2), :], in_=ps[bb_][:], func=AF.Tanh, scale=0.5))
                    S.op("dve", [PS(ba), ("rowb", 2 + (cc % 2))], [("ycv", cc)], lambda e, ba=ba, cc=cc: e.scalar_tensor_tensor(
                        out=ycv[:, cc, HALO:HALO + TT], in0=rowb[:, 2 + (cc % 2), :], scalar=1.0, in1=ps[ba][:],
                        op0=ALU.add, op1=ALU.mult))
                release()
            for cc in range(NCH):
                dset = cc % 2
                dgc = [PL(DG0 + 8 * dset + i) for i in range(8)]
                dgv = pool[:, DG0 + 8 * dset: DG0 + 8 * dset + 8, :].rearrange("p c t -> p (c t)")
                S.op("pool" if t > 0 else "dve", ["identb"] + VECS, dgc, lambda e, cc=cc, dgv=dgv: e.tensor_tensor(
                    out=dgv[:, 0:CW * 128].rearrange("p (k j) -> p k j", j=128),
                    in0=identb[:].unsqueeze(1).broadcast_to([128, CW, 128]),
                    in1=vecT[:, cc, 0:CW].unsqueeze(2).broadcast_to([128, CW, 128]), op=ALU.mult))
                bc = mmbank()

                def emit_cv(e, cc=cc, dgv=dgv, bc=bc):
                    ins = None
                    for k in range(CW):
                        ins = e.matmul(out=ps[bc][:], lhsT=dgv[:, k * 128:(k + 1) * 128], rhs=ycv[:, cc, k:k + TT],
                                       start=(k == 0), stop=(k == CW - 1))
                    return ins
                S.op("pe", dgc + [("ycv", cc), ("ycvh", cc)], [PS(bc)], emit_cv)
                S.op("dve", [PS(bc)] + VECS, [FT(cc)], lambda e, cc=cc, bc=bc: e.tensor_scalar(
                    out=ft[:, cc, :], in0=ps[bc][:], scalar1=0.5, scalar2=vcol("conv_dw_b", cc), op0=ALU.mult, op1=ALU.add))
                S.op("act", [FT(cc)], [PL(YACT0 + cc)], lambda e, cc=cc: e.activation(
                    out=pool[:, YACT0 + cc, :], in_=ft[:, cc, :], func=AF.Copy))
                S.op("act", [FT(cc)], [PL(SQ0 + cc)], lambda e, cc=cc: e.activation(
                    out=pool[:, SQ0 + cc, :], in_=ft[:, cc, :], func=AF.Square))
            for cc in range(NCH):
                if t > 0:
                    S.op("pool", [("ycv", cc)], [("ycvh", cc)], lambda e, cc=cc: e.tensor_copy(
                        out=ycv[:, cc, 0:HALO], in_=ycv[:, cc, TT:TT + HALO]))
                else:
                    S.op("act", [("ycv", cc)], [("ycvh", cc)], lambda e, cc=cc: e.activation(
                        out=ycv[:, cc, 0:HALO], in_=ycv[:, cc, TT:TT + HALO], func=AF.Copy))
            bm = mmbank()
            S.op("pe", [PL(YACT0 + c) for c in range(NCH)] + ["ones_s"], [PS(bm)],
                 lambda e, bm=bm: [e.matmul(out=ps[bm][:], lhsT=ones_s[:], rhs=pool[:, YACT0 + c, :],
                                     start=(c == 0), stop=(c == NCH - 1)) for c in range(NCH)][-1])
            S.op("pe", [PL(SQ0 + c) for c in range(NCH)] + ["ones_s"], [PS(ST_BANK)],
                 lambda e: [e.matmul(out=ps[ST_BANK][:], lhsT=ones_s[:], rhs=pool[:, SQ0 + c, :],
                                     start=(c == 0), stop=(c == NCH - 1)) for c in range(NCH)][-1])
            S.op("act", [PS(bm)], [("rowb", 1)], lambda e, bm=bm: e.activation(out=rowb[:, 1, :], in_=ps[bm][:], func=AF.Copy))
            S.op("dve", [("rowb", 1)], [("rowb", 2)], lambda e: e.tensor_tensor(
                out=rowb[:, 2, :], in0=rowb[:, 1, :], in1=rowb[:, 1, :], op=ALU.mult))
            S.op("dve", [PS(ST_BANK), ("rowb", 2)], [("rowb", 0)], lambda e: e.tensor_tensor(
                out=rowb[:, 0, :], in0=ps[ST_BANK][:], in1=rowb[:, 2, :], op=ALU.subtract))
            S.op("act", [("rowb", 0)], [("rowb", 0)], lambda e: e.activation(
                out=rowb[:, 0, :], in_=rowb[:, 0, :], func=AF.Sqrt, bias=EPS))
            S.op("dve", [("rowb", 0)], [("rowb", 0)], lambda e: e.reciprocal(out=rowb[:, 0, :], in_=rowb[:, 0, :]))
            def v_proj(half):
                s_v = next_slab("w_mix_in", MIX_ORDER.index("v%d" % half))
                for bk_ in range(4):
                    b = mmbank()

                    def emit_v(e, bk_=bk_, b=b, s_v=s_v):
                        ins = None
                        for kc in range(8):
                            ins = e.matmul(out=ps[b][:], lhsT=pool[:, U0 + kc, bk_ * 128:(bk_ + 1) * 128],
                                           rhs=ring[:, s_v, kc * 512:(kc + 1) * 512], start=(kc == 0), stop=(kc == 7))
                        return ins
                    S.op("pe", [("ring", s_v)] + UREG, [PS(b)], emit_v)
                    if bk_ % 2:
                        S.op("act", [PS(b)], [("vtok", bk_, half)], lambda e, bk_=bk_, b=b, half=half: e.activation(
                            out=vtok[:, bk_, half * 512:(half + 1) * 512], in_=ps[b][:], func=AF.Copy))
                    else:
                        S.op("dve", [PS(b)], [("vtok", bk_, half)], lambda e, bk_=bk_, b=b, half=half: e.tensor_copy(
                            out=vtok[:, bk_, half * 512:(half + 1) * 512], in_=ps[b][:]))
                release()

            for hvx in range(2):
                v_proj(hvx)
                for cc in range(4 * hvx, 4 * hvx + 4):
                    S.op("dve", [FT(cc), ("rowb", 1)], [FT(cc)], lambda e, cc=cc: e.tensor_tensor(
                        out=ft[:, cc, :], in0=ft[:, cc, :], in1=rowb[:, 1, :], op=ALU.subtract))
                for cc in range(4 * hvx, 4 * hvx + 4):
                    S.op("dve", [FT(cc), ("rowb", 0)], [FT(cc)], lambda e, cc=cc: e.tensor_tensor(
                        out=ft[:, cc, :], in0=ft[:, cc, :], in1=rowb[:, 0, :], op=ALU.mult))
                    S.op("act", [FT(cc)] + VECS, [PL(YACT0 + cc)], lambda e, cc=cc: e.activation(
                        out=pool[:, YACT0 + cc, :], in_=ft[:, cc, :], func=AF.Silu,
                        scale=vcol("conv_ln_g", cc), bias=vcol("conv_ln_b", cc)))
            YACT = [PL(YACT0 + c) for c in range(NCH)]
            for half in range(2):
                sg = next_slab("w_mix_in", MIX_ORDER.index("ga%d" % half))
                sp_ = next_slab("conv_w_pw", half)
                for s in range(4):
                    m = half * 4 + s
                    bg, by = mmbank(), mmbank()
                    proj_fm(sg, s * 128, range(8), lambda kc: pool[:, U0 + kc, :], UREG, bg, 512)
                    proj_fm(sp_, s * 128, range(8), lambda kc: pool[:, YACT0 + kc, :], YACT, by, 512)
                    S.op("act", [PS(bg)], [("rowb", 2 + (m % 2))], lambda e, bg=bg, m=m: e.activation(
                        out=rowb[:, 2 + (m % 2), :], in_=ps[bg][:], func=AF.Tanh, scale=0.5))
                    S.op("dve", [PS(by), ("rowb", 2 + (m % 2))], [PL(MRG0 + m)], lambda e, by=by, m=m: e.scalar_tensor_tensor(
                        out=pool[:, MRG0 + m, :], in0=rowb[:, 2 + (m % 2), :], scalar=1.0, in1=ps[by][:],
                        op0=ALU.add, op1=ALU.mult))
                release()

            s_lr = next_slab("w_mix_in", MIX_ORDER.index("lr"))
            b = mmbank()

            def emit_lr(e, s_lr=s_lr, b=b):
                ins = None
                for kc in range(8):
                    ins = e.matmul(out=ps[b][0:16, :], lhsT=ring[:, s_lr, kc * 16:(kc + 1) * 16], rhs=pool[:, U0 + kc, :],
                                   start=(kc == 0), stop=(kc == 7))
                return ins
            S.op("pe", [("ring", s_lr)] + UREG, [PS(b)], emit_lr)
            S.op("dve", [PS(b)], ["lrT"], lambda e, b=b: e.tensor_copy(out=lrT[0:16, :], in_=ps[b][0:16, :]))
            release()
            s_q = next_slab("w_mix_in", MIX_ORDER.index("q"))
            s_k = next_slab("w_mix_in", MIX_ORDER.index("k"))
            for hd in range(4):
                b = mmbank()
                S.op("pe", ["wal_a", "wal_b", "lrT"], [PS(b)], lambda e, hd=hd, b=b: e.matmul(
                    out=ps[b][:], lhsT=wal[0:17, hd * 128:(hd + 1) * 128], rhs=lrT[0:17, :], start=True, stop=True))
                S.op("act", [PS(b)], [("rowb", 0)], lambda e, b=b: e.activation(out=rowb[:, 0, :], in_=ps[b][:], func=AF.Exp, scale=-1.0))
                S.op("act", [("rowb", 0)], [("rowb", 0)], lambda e: e.activation(out=rowb[:, 0, :], in_=rowb[:, 0, :], func=AF.Ln, bias=1.0))
                S.op("dve", [("rowb", 0), "rmask"], [("rowb", 1)], lambda e: e.tensor_tensor_scan(
                    out=rowb[:, 1, :], data0=rmask[:], data1=rowb[:, 0, :], initial=0.0, op0=ALU.mult, op1=ALU.add))
                e1 = 2 + (hd % 2)
                S.op("act", [("rowb", 1)], [("rowb", e1)], lambda e, e1=e1: e.activation(
                    out=rowb[:, e1, :], in_=rowb[:, 1, :], func=AF.Exp, scale=-1.0 / 16.0))
                S.op("act", [("rowb", 1)], ["gl4"], lambda e: e.activation(
                    out=gl4[:], in_=rowb[:, 1, :], func=AF.Exp, scale=1.0 / 16.0))
                bq, bk = mmbank(), mmbank()
                proj_fm(s_q, hd * 128, range(8), lambda kc: pool[:, U0 + kc, :], UREG, bq, 512)
                proj_fm(s_k, hd * 128, range(8), lambda kc: pool[:, U0 + kc, :], UREG, bk, 512)
                S.op("dve", [PS(bq), ("rowb", e1)], [PL(QD0 + hd)], lambda e, bq=bq, hd=hd, e1=e1: e.scalar_tensor_tensor(
                    out=pool[:, QD0 + hd, :], in0=ps[bq][:], scalar=128.0 ** -0.5, in1=rowb[:, e1, :], op0=ALU.mult, op1=ALU.mult))
                S.op("dve", [PS(bk), "gl4"], [PL(KI0 + hd)], lambda e, bk=bk, hd=hd: e.tensor_tensor(
                    out=pool[:, KI0 + hd, :], in0=ps[bk][:], in1=gl4[:], op=ALU.mult))
                S.op("dve", [("rowb", e1)], [("alast", hd)], lambda e, hd=hd, e1=e1: e.tensor_copy(
                    out=alast[:, hd, 0:4], in_=rowb[:, e1, :].rearrange("p (c k) -> p c k", k=128)[:, :, 127]))
            for bk_ in range(4):
                b = mmbank()
                S.op("pe", ["wal_a", "wal_b", "lrT"], [PS(b)], lambda e, bk_=bk_, b=b: e.matmul(
                    out=ps[b][:], lhsT=lrT[0:17, bk_ * 128:(bk_ + 1) * 128], rhs=wal[0:17, :], start=True, stop=True))
                S.op("act", [PS(b)], [("rowb", 0)], lambda e, b=b: e.activation(out=rowb[:, 0, :], in_=ps[b][:], func=AF.Exp, scale=-1.0))
                S.op("act", [("rowb", 0)], [("rowb", 0)], lambda e: e.activation(out=rowb[:, 0, :], in_=rowb[:, 0, :], func=AF.Ln, bias=1.0))
                b2 = mmbank()
                S.op("pe", [("rowb", 0), "triU"], [PS(b2)], lambda e, b2=b2: e.matmul(
                    out=ps[b2][:], lhsT=triU[:], rhs=rowb[:, 0, :], start=True, stop=True))
                S.op("act", [PS(b2)], [("rowb", 1)], lambda e, b2=b2: e.activation(
                    out=rowb[:, 1, :], in_=ps[b2][:], func=AF.Exp, scale=-1.0 / 16.0))
                b3 = mmbank()

                def emit_kt(e, bk_=bk_, b3=b3, s_k=s_k):
                    ins = None
                    for kc in range(8):
                        ins = e.matmul(out=ps[b3][:], lhsT=pool[:, U0 + kc, bk_ * 128:(bk_ + 1) * 128],
                                       rhs=ring[:, s_k, kc * 512:(kc + 1) * 512], start=(kc == 0), stop=(kc == 7))
                    return ins
                S.op("pe", [("ring", s_k)] + UREG, [PS(b3)], emit_kt)
                S.op("dve", [PS(b3), ("rowb", 1)], [("kdtok", bk_)], lambda e, bk_=bk_, b3=b3: e.tensor_tensor(
                    out=kdtok[:, bk_, :], in0=ps[b3][:], in1=rowb[:, 1, :], op=ALU.mult))
            release()
            release()
            for half in range(2):
                s_g = next_slab("w_mix_in", MIX_ORDER.index("g%d" % half))
                for s in range(4):
                    m = half * 4 + s
                    b = mmbank()
                    proj_fm(s_g, s * 128, range(8), lambda kc: pool[:, U0 + kc, :], UREG, b, 512)
                    S.op("act", [PS(b)], [PL(GS0 + m)], lambda e, b=b, m=m: e.activation(
                        out=pool[:, GS0 + m, :], in_=ps[b][:], func=AF.Silu))
                release()
            SCB = [2, 3]
            DLB = [0, 1]
            OBK = {0: (4, 5), 1: (6, 7)}

            def obank(bk_, hd):
                return OBK[bk_ % 2][hd // 2]

            def ocol(hd, ec):
                return (hd % 2) * 256 + ec * 128

            def stage_a(bk_):
                tsl = slice(bk_ * 128, (bk_ + 1) * 128)
                for bb in OBK[bk_ % 2]:
                    S.op("pe", ["zerob", PL(U0)], [PS(bb)], lambda e, bb=bb: e.matmul(
                        out=ps[bb][:], lhsT=zerob[:], rhs=pool[:, U0, :], start=True, stop=False))
                for hd in range(4):
                    sb_ = SCB[hd % 2]
                    S.op("pe", [PL(KI0 + hd), PL(QD0 + hd)], [PS(sb_)], lambda e, hd=hd, tsl=tsl, sb_=sb_: e.matmul(
                        out=ps[sb_][:, 0:128], lhsT=pool[:, KI0 + hd, tsl], rhs=pool[:, QD0 + hd, tsl], start=True, stop=True))
                    S.op("dve", [PS(sb_), "maskT"], [("scm", hd)], lambda e, hd=hd, sb_=sb_: e.tensor_tensor(
                        out=scm[:, hd, :], in0=ps[sb_][:, 0:128], in1=maskT[:], op=ALU.mult))
                for hd in range(4):
                    ob_ = obank(bk_, hd)

                    def emit_i(e, hd=hd, bk_=bk_, tsl=tsl, ob_=ob_):
                        ins = None
                        for ec in range(2):
                            ins = e.matmul(out=ps[ob_][:, ocol(hd, ec):ocol(hd, ec) + 128],
                                           lhsT=vtok[:, bk_, hd * 256 + ec * 128: hd * 256 + ec * 128 + 128],
                                           rhs=scm[:, hd, :], start=False, stop=False)
                        return ins
                    S.op("pe", [("scm", hd)] + [("vtok", bk_, i) for i in range(2)], [PS(ob_)], emit_i)

            def stage_b(bk_):
                par = bk_ % 2
                tsl = slice(bk_ * 128, (bk_ + 1) * 128)
                for hd in range(4):
                    ob_ = obank(bk_, hd)

                    def emit_x(e, hd=hd, ob_=ob_, tsl=tsl, par=par):
                        ins = None
                        for ec in range(2):
                            o0 = ocol(hd, ec)
                            ins = e.matmul(out=ps[ob_][:, o0:o0 + 128], lhsT=Sb[:, hd * 2 + par, ec * 128:(ec + 1) * 128],
                                           rhs=pool[:, QD0 + hd, tsl], start=False, stop=True)
                        return ins
                    S.op("pe", [("Sb", hd * 2 + par), PL(QD0 + hd)], [PS(ob_)], emit_x)
                for hd in range(4):
                    db_ = DLB[hd % 2]
                    S.op("pe", [("kdtok", bk_)] + [("vtok", bk_, i) for i in range(2)], [PS(db_)],
                         lambda e, hd=hd, bk_=bk_, db_=db_: e.matmul(
                             out=ps[db_][:, 0:256], lhsT=kdtok[:, bk_, hd * 128:(hd + 1) * 128],
                             rhs=vtok[:, bk_, hd * 256:(hd + 1) * 256], start=True, stop=True))
                    S.op("dve", [PS(db_), ("Sst", hd), ("alast", hd)], [("Sst", hd)],
                         lambda e, hd=hd, bk_=bk_, db_=db_: e.scalar_tensor_tensor(
                             out=Sst[:, hd, :], in0=Sst[:, hd, :], scalar=alast[:, hd, bk_:bk_ + 1], in1=ps[db_][:, 0:256],
                             op0=ALU.mult, op1=ALU.add))
                    if hd % 2 and t > 0:
                        S.op("pool", [("Sst", hd)], [("Sb", hd * 2 + (1 - par))], lambda e, hd=hd, par=par: e.tensor_copy(
                            out=Sb[:, hd * 2 + (1 - par), :], in_=Sst[:, hd, :]))
                    else:
                        S.op("act", [("Sst", hd)], [("Sb", hd * 2 + (1 - par))], lambda e, hd=hd, par=par: e.activation(
                            out=Sb[:, hd * 2 + (1 - par), :], in_=Sst[:, hd, :], func=AF.Copy))

            def evac(bk_):
                for i, bb in enumerate(OBK[bk_ % 2]):
                    S.op("act", [PS(bb)], [FT(4 * i + j) for j in range(4)], lambda e, i=i, bb=bb, bk_=bk_: e.activation(
                        out=ft[:, 4 * i:4 * i + 4, bk_ * 128:(bk_ + 1) * 128],
                        in_=ps[bb][:].rearrange("p (m t) -> p m t", m=4), func=AF.Copy))

            stage_a(0)
            for bk_ in range(4):
                if bk_ + 1 < 4:
                    stage_a(bk_ + 1)
                stage_b(bk_)
                evac(bk_)
            TG0 = 32

            def gb_half(half):
                sg = next_slab("w_mix_in", MIX_ORDER.index("gb%d" % half))
                for s_ in range(4):
                    m = half * 4 + s_
                    bg = mmbank()
                    proj_fm(sg, s_ * 128, range(8), lambda kc: pool[:, U0 + kc, :], UREG, bg, 512)
                    S.op("act", [PS(bg)], [PL(TG0 + m)], lambda e, bg=bg, m=m: e.activation(
                        out=pool[:, TG0 + m, :], in_=ps[bg][:], func=AF.Tanh, scale=0.5))
                release()

            for m in range(NCH):
                S.op("act", [FT(m)], [PL(SQ0 + m)], lambda e, m=m: e.activation(
                    out=pool[:, SQ0 + m, :], in_=ft[:, m, :], func=AF.Square))
            gb_half(0)
            NB_ = [2, 3, 0, 1]
            for hd in range(4):
                nb_ = NB_[hd]
                S.op("pe", [PL(SQ0 + 2 * hd), PL(SQ0 + 2 * hd + 1), "ones_s"], [PS(nb_)], lambda e, hd=hd, nb_=nb_: [e.matmul(
                    out=ps[nb_][:], lhsT=ones_s[:], rhs=pool[:, SQ0 + 2 * hd + ec, :], start=(ec == 0), stop=(ec == 1)) for ec in range(2)][-1])
            for hd in range(4):
                nb_ = NB_[hd]
                S.op("act", [PS(nb_)], [("rowb", hd)], lambda e, hd=hd, nb_=nb_: e.activation(
                    out=rowb[:, hd, :], in_=ps[nb_][:], func=AF.Sqrt, scale=4.0, bias=EPS))
            for hd in range(4):
                S.op("dve", [("rowb", hd)], [("rowb", hd)], lambda e, hd=hd: e.reciprocal(out=rowb[:, hd, :], in_=rowb[:, hd, :]))
            gb_half(1)
            for hd in range(4):
                for ec in range(2):
                    m = hd * 2 + ec
                    S.op("dve", [FT(m), ("rowb", hd)] + VECS, [FT(m)], lambda e, m=m, hd=hd: e.scalar_tensor_tensor(
                        out=ft[:, m, :], in0=ft[:, m, :], scalar=vcol("gla_norm", m), in1=rowb[:, hd, :],
                        op0=ALU.mult, op1=ALU.mult))
                for ec in range(2):
                    m = hd * 2 + ec
                    S.op("pool" if t > 0 else "dve", [FT(m), PL(GS0 + m)], [PL(OG0 + m)], lambda e, m=m: e.tensor_tensor(
                        out=pool[:, OG0 + m, :], in0=ft[:, m, :], in1=pool[:, GS0 + m, :], op=ALU.mult))
            OG = [PL(OG0 + c) for c in range(NCH)]
            for half in range(2):
                so = next_slab("gla_w_o", half)
                for s_ in range(4):
                    m = half * 4 + s_
                    by = mmbank()
                    proj_fm(so, s_ * 128, range(8), lambda kc: pool[:, OG0 + kc, :], OG, by, 512)
                    S.op("dve", [PS(by), PL(TG0 + m)], [("rowb", m % 4)], lambda e, by=by, m=m: e.scalar_tensor_tensor(
                        out=rowb[:, m % 4, :], in0=pool[:, TG0 + m, :], scalar=1.0, in1=ps[by][:],
                        op0=ALU.add, op1=ALU.mult))
                    S.op("pool" if t > 0 else "dve", [("rowb", m % 4), PL(MRG0 + m)], [PL(MRG0 + m)], lambda e, m=m: e.tensor_tensor(
                        out=pool[:, MRG0 + m, :], in0=rowb[:, m % 4, :], in1=pool[:, MRG0 + m, :], op=ALU.add))
                release()
            MRG = [PL(MRG0 + c) for c in range(NCH)]
            for half in range(2):
                so = next_slab("w_mix_out", half)
                for s in range(4):
                    m = half * 4 + s
                    b = mmbank()
                    proj_fm(so, s * 128, range(8), lambda kc: pool[:, MRG0 + kc, :], MRG, b, 512)
                    S.op("dve", [PS(b), Hk(m)], [Hk(m)], lambda e, b=b, m=m, hm=hv(m): e.scalar_tensor_tensor(
                        out=hm, in0=ps[b][:], scalar=0.5, in1=hm, op0=ALU.mult, op1=ALU.add))
                    h_updated(m)
                release()
            if t == 0:
                dump(2)

            ffn("ffn2_w_in", "ffn2_w_out", "ffn2_norm")
            if t == 0:
                dump(3)

            spp = next_slab("ple_w_proj", 0)
            PT = [("pT", 0), ("pT", 1)]
            for m in range(NCH):
                bp = mmbank()
                proj_fm(spp, m * 128, range(2), lambda kc: pT[:, kc, :], PT, bp, 1024)
                S.op("act", [PS(bp)], [FT(m)], lambda e, bp=bp, m=m: e.activation(out=ft[:, m, :], in_=ps[bp][:], func=AF.Copy))
            release()
            rmsnorm_h_to_u("ple_norm")
            sg0 = next_slab("ple_w_gate", 0)
            sg1 = next_slab("ple_w_gate", 1)
            for m in range(NCH):
                sg = sg0 if m < 4 else sg1
                s_ = m % 4
                bg = mmbank()
                proj_fm(sg, s_ * 128, range(8), lambda kc: pool[:, U0 + kc, :], UREG, bg, 512, stream_first=(m == 0))
                S.op("act", [PS(bg)], [("rowb", 2 + (m % 2))], lambda e, bg=bg, m=m: e.activation(
                    out=rowb[:, 2 + (m % 2), :], in_=ps[bg][:], func=AF.Tanh, scale=0.5))
                S.op("dve", [FT(m), ("rowb", 2 + (m % 2))], [FT(m)], lambda e, m=m: e.scalar_tensor_tensor(
                    out=ft[:, m, :], in0=rowb[:, 2 + (m % 2), :], scalar=1.0, in1=ft[:, m, :], op0=ALU.add, op1=ALU.mult))
            release()
            release()
            rms_stats(ALLFT, lambda c: ft[:, c, :], NCH, 0.25, 1)
            for m in range(NCH):
                S.op("pool" if t > 0 else "dve", [FT(m), ("rowb", 1)], [FT(m)], lambda e, m=m: e.tensor_tensor(
                    out=ft[:, m, :], in0=ft[:, m, :], in1=rowb[:, 1, :], op=ALU.mult))
                S.op("dve", [FT(m), Hk(m)] + VECS, [Hk(m)], lambda e, m=m, hm=hv(m): e.scalar_tensor_tensor(
                    out=hm, in0=ft[:, m, :], scalar=vecT[:, m, 63:64], in1=hm, op0=ALU.mult, op1=ALU.add))
                h_updated(m)
            if t == 0:
                dump(4)

            h_stats_finish(0)
            for c in range(NCH):
                S.op("dve", [Hk(c), ("rowb", 0)] + VECS, [FT(c)], lambda e, c=c, hm=hv(c): e.scalar_tensor_tensor(
                    out=ft[:, c, :], in0=hm, scalar=vcol("final_norm", c), in1=rowb[:, 0, :],
                    op0=ALU.mult, op1=ALU.mult))
            pending_out.append(t)
            if not DEFER_OUT:
                emit_out_ref[0](t)

        if DEFER_OUT:
            emit_out(NT - 1)
        S.final_wait("sp", out_evs)
        assert st_used[0] == len(stream)
        S.emit_all(block)
    return nc


_W_KEYS = ["ffn1_w_in", "ffn1_w_out", "w_mix_in", "conv_w_pw", "gla_w_o", "w_mix_out",
           "ffn2_w_in", "ffn2_w_out", "ple_w_gate", "ple_w_proj"]
_V_KEYS = ["ffn1_norm", "mix_norm", "conv_dw_b", "conv_ln_g", "conv_ln_b", "gla_norm",
           "ffn2_norm", "ple_norm", "ple_post_norm"]


def make_in_maps(inputs, n_cores, T):
    f = lambda a: np.ascontiguousarray(np.asarray(a, dtype=np.float32))
    shared = {}
    for k in _W_KEYS:
        shared[k] = f(inputs[k])[0]
    for k in _V_KEYS:
        shared[k] = f(inputs[k]).reshape(1, D)
    shared["final_norm"] = f(inputs["final_norm"]).reshape(1, D)
    shared["conv_dw_w"] = f(inputs["conv_dw_w"])[0]
    shared["gla_w_alpha"] = f(inputs["gla_w_alpha"])[0]
    shared["gla_b_alpha"] = f(inputs["gla_b_alpha"]).reshape(1, 512)
    x = f(inputs["x"])
    p = f(inputs["p"])[0]
    maps = []
    for c in range(n_cores):
        m = dict(shared)
        m["x"] = np.ascontiguousarray(x[c, :T])
        m["p"] = np.ascontiguousarray(p[c, :T])
        maps.append(m)
    return maps


def kernel(**inputs):
    B, T, _ = inputs["x"].shape
    nc = build_program(T)
    in_maps = make_in_maps(inputs, B, T)
    res = run_bass_kernel_spmd(nc, in_maps, core_ids=list(range(B)))
    out = np.stack([np.asarray(r["y"], dtype=np.float32) for r in res.results], axis=0)
    return out
```

```python
import contextlib
import numpy as np
import concourse.bass as bass
import concourse.mybir as mybir
from concourse.bass_utils import run_bass_kernel_spmd

F32 = mybir.dt.float32
BF16 = mybir.dt.bfloat16
AF = mybir.ActivationFunctionType
ALU = mybir.AluOpType

D = 1024
DFF = 2816
NIN = 7184
PLE = 256
CW = 31
HALO = CW - 1
TT = 512
EPS = 1e-6
NCH = D // 128
NHC = DFF // 128
RING = 5
SLOT = 4096
NPOOL = 48
DEFER_OUT = True
STRICT_SAME_ENGINE = True

ENG_NAMES = ("pe", "act", "dve", "pool", "sp")


class Sched:
    def __init__(self, nc, stack):
        self.nc = nc
        self.stack = stack
        self.prog = {e: [] for e in ENG_NAMES}
        self.count = {e: 0 for e in ENG_NAMES}
        self.known = {e: {} for e in ENG_NAMES}
        self.sems = {}
        self.dma_count = {}
        self.last_write = {}
        self.readers = {}
        for e in ENG_NAMES:
            self.sem(e)

    def sem(self, name):
        if name not in self.sems:
            self.sems[name] = self.stack.enter_context(self.nc.semaphore("s_" + name))
        return self.sems[name]

    def _deps(self, reads, writes, extra):
        deps = set(extra)
        for r in reads:
            if r in self.last_write:
                deps.add(self.last_write[r])
            if isinstance(r, tuple) and r[0] == "ps":
                for ev in self.readers.get(r, ()):
                    deps.add(ev)
        for w in writes:
            if w in self.last_write:
                deps.add(self.last_write[w])
            for ev in self.readers.get(w, ()):
                deps.add(ev)
        return deps

    def _waits(self, eng, deps):
        need = {}
        for (k, v) in deps:
            if k == eng:
                if eng in ("pe", "sp"):
                    continue
                if (not STRICT_SAME_ENGINE) and eng != "pool" and v < self.count[eng]:
                    continue
            if self.known[eng].get(k, 0) >= v:
                continue
            need[k] = max(need.get(k, 0), v)
        for k, v in need.items():
            self.known[eng][k] = v
        return [(self.sems[k], v) for k, v in need.items()]

    def _record(self, ev, reads, writes):
        for w in writes:
            self.last_write[w] = ev
            self.readers[w] = []
        for r in reads:
            if r not in writes:
                self.readers.setdefault(r, []).append(ev)

    def op(self, eng, reads, writes, emit, extra=()):
        reads = list(reads)
        writes = list(writes)
        waits = self._waits(eng, self._deps(reads, writes, extra))
        self.count[eng] += 1
        ev = (eng, self.count[eng])
        sem = self.sems[eng]

        def run(e, waits=waits, emit=emit, sem=sem):
            for s, v in waits:
                e.wait_ge(s, v)
            ins = emit(e)
            ins.then_inc(sem, 1)

        self.prog[eng].append(run)
        self._record(ev, reads, writes)
        return ev

    def dma(self, eng, semname, reads, writes, emit, extra=()):
        reads = list(reads)
        writes = list(writes)
        self.sem(semname)
        waits = self._waits(eng, self._deps(reads, writes, extra))
        self.dma_count[semname] = self.dma_count.get(semname, 0) + 1
        ev = (semname, 16 * self.dma_count[semname])
        sem = self.sems[semname]

        def run(e, waits=waits, emit=emit, sem=sem):
            for s, v in waits:
                e.wait_ge(s, v)
            ins = emit(e)
            ins.then_inc(sem, 16)

        self.prog[eng].append(run)
        self._record(ev, reads, writes)
        return ev

    def final_wait(self, eng, events):
        waits = self._waits(eng, set(events))

        def run(e, waits=waits):
            for s, v in waits:
                e.wait_ge(s, v)

        self.prog[eng].append(run)

    def emit_all(self, block):
        progs = self.prog

        @block.tensor
        def _(e):
            for f in progs["pe"]:
                f(e)

        @block.scalar
        def _(e):
            for f in progs["act"]:
                f(e)

        @block.vector
        def _(e):
            for f in progs["dve"]:
                f(e)

        @block.gpsimd
        def _(e):
            for f in progs["pool"]:
                f(e)

        @block.sync
        def _(e):
            for f in progs["sp"]:
                f(e)


MIX_SLABS = {
    "ca0": (0, 512), "ca1": (512, 512), "cb0": (1024, 512), "cb1": (1536, 512),
    "q": (2048, 512), "k": (2560, 512), "v0": (3072, 512), "v1": (3584, 512),
    "g0": (4096, 512), "g1": (4608, 512), "lr": (5120, 16),
    "ga0": (5136, 512), "ga1": (5648, 512), "gb0": (6160, 512), "gb1": (6672, 512),
}
MIX_ORDER = list(MIX_SLABS.keys())

WEIGHT_NAMES = ["ffn1_w_in", "ffn1_w_out", "w_mix_in", "conv_w_pw", "gla_w_o", "w_mix_out",
                "ffn2_w_in", "ffn2_w_out", "ple_w_gate", "ple_w_proj"]


def slab_table():
    tab = {}
    for nm in ("ffn1_w_in", "ffn2_w_in"):
        sl = []
        for j in range(11):
            pieces = []
            for g in range(2):
                pieces.append((g * 256, 0, 8, g * DFF + j * 256, 256, 512))
            sl.append((8 * 512, pieces))
        tab[nm] = sl
    for nm in ("ffn1_w_out", "ffn2_w_out"):
        sl = []
        for s in range(8):
            cg, half = s // 2, s % 2
            sl.append((11 * 256, [(0, half * 11, 11, cg * 256, 256, 256)]))
        tab[nm] = sl
    sl = []
    for nm in MIX_ORDER:
        c0, ncol = MIX_SLABS[nm]
        sl.append((8 * ncol, [(0, 0, 8, c0, ncol, ncol)]))
    tab["w_mix_in"] = sl
    for nm in ("conv_w_pw", "gla_w_o", "w_mix_out", "ple_w_gate"):
        tab[nm] = [(8 * 512, [(0, 0, 8, s * 512, 512, 512)]) for s in range(2)]
    tab["ple_w_proj"] = [(2 * 1024, [(0, 0, 2, 0, 1024, 1024)])]
    return tab


def build_program(T, dbg=False):
    NT = T // TT
    nc = bass.Bass("TRN2", target_bir_lowering=False)
    tab = slab_table()

    def din(name, shape):
        return nc.dram_tensor(name, list(shape), F32, kind="ExternalInput").ap()

    x_d = din("x", (T, D))
    p_d = din("p", (T, PLE))
    W = {}
    W["ffn1_w_in"] = din("ffn1_w_in", (D, 2 * DFF))
    W["ffn1_w_out"] = din("ffn1_w_out", (DFF, D))
    W["w_mix_in"] = din("w_mix_in", (D, NIN))
    W["conv_w_pw"] = din("conv_w_pw", (D, D))
    W["gla_w_o"] = din("gla_w_o", (D, D))
    W["w_mix_out"] = din("w_mix_out", (D, D))
    W["ffn2_w_in"] = din("ffn2_w_in", (D, 2 * DFF))
    W["ffn2_w_out"] = din("ffn2_w_out", (DFF, D))
    W["ple_w_gate"] = din("ple_w_gate", (D, D))
    W["ple_w_proj"] = din("ple_w_proj", (PLE, D))
    VEC_NAMES = ["ffn1_norm", "mix_norm", "conv_dw_b", "conv_ln_g", "conv_ln_b", "gla_norm",
                 "ffn2_norm", "ple_norm", "ple_post_norm", "final_norm"]
    V = {n: din(n, (1, D)) for n in VEC_NAMES}
    dww_d = din("conv_dw_w", (CW, D))
    walpha_d = din("gla_w_alpha", (16, 512))
    balpha_d = din("gla_b_alpha", (1, 512))
    y_d = nc.dram_tensor("y", [T, D], F32, kind="ExternalOutput").ap()
    dbg_d = None
    if dbg:
        dbg_d = nc.dram_tensor("dbg", [8, 128, NCH, TT], F32, kind="ExternalOutput").ap()

    scr = {}
    for nm in WEIGHT_NAMES:
        nsl = len(tab[nm])
        scr[nm] = nc.dram_tensor("scr_" + nm, [nsl, 128, SLOT], BF16, kind="Internal").ap()

    with contextlib.ExitStack() as st:
        def sb(name, shape, dt):
            return st.enter_context(nc.sbuf_tensor(name, list(shape), dt))

        hh = sb("hh", (128, 2 * NCH, TT), F32)
        xin = sb("xin", (128, 4, D), F32)
        pin = sb("pin", (128, 4, PLE), F32)
        ft = sb("ft", (128, NCH, TT), F32)
        pool = sb("pool", (128, NPOOL, TT), BF16)
        ycv = sb("ycv", (128, NCH, HALO + TT), BF16)
        vtok = sb("vtok", (128, 4, D), BF16)
        kdtok = sb("kdtok", (128, 4, 512), BF16)
        ring = sb("ring", (128, RING, SLOT), BF16)
        rowb = sb("rowb", (128, 4, TT), F32)
        gl4 = sb("gl4", (128, TT), F32)
        Sst = sb("Sst", (128, 4, 256), F32)
        Sb = sb("Sb", (128, 8, 256), BF16)
        scm = sb("scm", (128, 4, 128), BF16)
        zerob = sb("zerob", (128, 128), BF16)
        identf = sb("identf", (128, 128), F32)
        onesf = sb("onesf", (128, 128), F32)
        identb = sb("identb", (128, 128), BF16)
        ones_s = sb("ones_s", (128, 128), BF16)
        maskT = sb("maskT", (128, 128), F32)
        triU = sb("triU", (128, 128), F32)
        rmask = sb("rmask", (128, TT), F32)
        vecT = sb("vecT", (128, NCH, 64), F32)
        wal = sb("wal", (32, 512), BF16)
        lrT = sb("lrT", (32, TT), BF16)
        pT = sb("pT", (128, 2, TT), BF16)
        alast = sb("alast", (128, 4, 8), F32)
        ps = [st.enter_context(nc.psum_tensor("ps%d" % i, [128, 512], F32)) for i in range(8)]

        S = Sched(nc, st)
        block = st.enter_context(nc.Block())

        mm_banks = [0, 1, 4, 5, 6, 7]
        mm_ctr = [0]

        def mmbank():
            b = mm_banks[mm_ctr[0] % len(mm_banks)]
            mm_ctr[0] += 1
            return b

        ST_BANK = 2
        SC_BANK = 3

        def PS(b):
            return ("ps", b)

        def PL(i):
            return ("pool", i)

        HP = [0]

        def Hk(i):
            return ("h", HP[0], i)

        def hv(c):
            return hh[:, HP[0] * NCH + c, :]

        def FT(i):
            return ("ft", i)

        def ALLH_():
            return [Hk(i) for i in range(NCH)]
        ALLFT = [FT(i) for i in range(NCH)]

        U0, SQ0, X0 = 0, 8, 16
        HID0 = 16
        YACT0, MRG0 = 16, 24
        DG0 = 32
        QD0, KI0, GS0, OG0 = 32, 36, 16, 40

        VC = {n: 32 + i for i, n in enumerate(VEC_NAMES)}

        def vcol(name, c):
            return vecT[:, c, VC[name]:VC[name] + 1]

        S.op("pool", [], ["identf"], lambda e: e.memset(identf[:], 0.0))
        S.op("pool", ["identf"], ["identf"], lambda e: e.affine_select(
            out=identf[:], in_=identf[:], pattern=[[-1, 128]], compare_op=ALU.not_equal, fill=1.0,
            base=0, channel_multiplier=1))
        S.op("pool", [], ["onesf"], lambda e: e.memset(onesf[:], 1.0))
        S.op("pool", [], ["zerob"], lambda e: e.memset(zerob[:], 0.0))
        S.op("pool", ["identf"], ["identb"], lambda e: e.tensor_copy(out=identb[:], in_=identf[:]))
        S.op("pool", [], ["ones_s"], lambda e: e.memset(ones_s[:], 1.0 / 1024.0))

        S.op("pool", ["onesf"], ["maskT"], lambda e: e.affine_select(
            out=maskT[:], in_=onesf[:], pattern=[[1, 128]], compare_op=ALU.is_ge, fill=0.0, base=0, channel_multiplier=-1))
        S.op("pool", ["onesf"], ["triU"], lambda e: e.affine_select(
            out=triU[:], in_=onesf[:], pattern=[[-1, 128]], compare_op=ALU.is_gt, fill=0.0, base=0, channel_multiplier=1))

        S.op("pool", [], ["rmask"], lambda e: e.memset(rmask[:], 1.0))
        S.op("pool", ["rmask"], ["rmask"], lambda e: e.memset(rmask[:].rearrange("p (c k) -> p c k", k=128)[:, :, 0:1], 0.0))
        S.op("pool", [], ["lrT"], lambda e: e.memset(lrT[:], 1.0))
        S.op("pool", [], ["Sst"], lambda e: e.memset(Sst[:], 0.0))
        S.op("pool", [], [("Sb", i) for i in range(8)], lambda e: e.memset(Sb[:], 0.0))
        S.op("pool", [], [("ycvh", c) for c in range(NCH)], lambda e: e.memset(ycv[:, :, 0:HALO], 0.0))
        xin_zero_ev = S.op("pool", [], ["xin"], lambda e: e.memset(xin[:, 0, :], 0.0))

        def ld_vecs(e):
            ins = e.dma_start(out=xin[0:CW, 0, :], in_=dww_d)
            return ins

        S.dma("pool", "cst_dw", [], ["xin_dw"], ld_vecs, extra=[xin_zero_ev])
        for i, n in enumerate(VEC_NAMES):
            S.dma("pool", "cst_v%d" % i, [], ["xin_v%d" % i],
                  (lambda e, i=i, n=n: e.dma_start(out=xin[32 + i:33 + i, 0, :], in_=V[n])), extra=[xin_zero_ev])
        S.dma("pool", "cst_wa", [], ["wal_a"], lambda e: e.dma_start(out=wal[0:16, :], in_=walpha_d))
        S.dma("pool", "cst_wb", [], ["wal_b"], lambda e: e.dma_start(out=wal[16:17, :], in_=balpha_d))

        for c in range(NCH):
            b = mmbank()
            S.op("pe", ["xin", "xin_dw", "identf"] + ["xin_v%d" % i for i in range(len(VEC_NAMES))], [PS(b)], lambda e, c=c, b=b: e.transpose(
                out=ps[b][:, 0:128], in_=xin[:, 0, c * 128:(c + 1) * 128], identity=identf[:]))
            S.op("dve", [PS(b)], [("vecT", c)], lambda e, c=c, b=b: e.tensor_copy(
                out=vecT[:, c, :], in_=ps[b][:, 0:64]))
            S.op("dve", [("vecT", c)], [("vecT", c)], lambda e, c=c: e.tensor_scalar(
                out=vecT[:, c, 63:64], in0=vecT[:, c, VC["ple_post_norm"]:VC["ple_post_norm"] + 1], scalar1=0.5, scalar2=None,
                op0=ALU.mult))
        VECS = [("vecT", c) for c in range(NCH)]

        stream = []
        for t in range(NT):
            for nm in ("ffn1_w_in", "ffn1_w_out"):
                for si in range(len(tab[nm])):
                    stream.append((nm, si))
            mo = {n: i for i, n in enumerate(MIX_ORDER)}
            for key in ("ca0", "cb0", "ca1", "cb1", "v0", "v1"):
                stream.append(("w_mix_in", mo[key]))
            stream += [("w_mix_in", mo["ga0"]), ("conv_w_pw", 0), ("w_mix_in", mo["ga1"]), ("conv_w_pw", 1)]
            for key in ("lr", "q", "k", "g0", "g1"):
                stream.append(("w_mix_in", mo[key]))
            stream += [("w_mix_in", mo["gb0"]), ("w_mix_in", mo["gb1"]), ("gla_w_o", 0), ("gla_w_o", 1)]
            stream += [("w_mix_out", 0), ("w_mix_out", 1)]
            for nm in ("ffn2_w_in", "ffn2_w_out"):
                for si in range(len(tab[nm])):
                    stream.append((nm, si))
            stream += [("ple_w_proj", 0), ("ple_w_gate", 0), ("ple_w_gate", 1)]
        st_loaded = [0]
        st_used = [0]

        def pump():
            while st_loaded[0] < len(stream) and st_loaded[0] < st_used[0] + RING:
                i = st_loaded[0]
                nm, si = stream[i]
                slot = i % RING
                elems = tab[nm][si][0]
                S.dma("sp", "ring%d" % slot, [], [("ring", slot)],
                      (lambda e, nm=nm, si=si, slot=slot, elems=elems: e.dma_start(
                          out=ring[:, slot, 0:elems], in_=scr[nm][si, :, 0:elems])),
                      extra=[cast_ev.get((nm, si), cast_ev[nm])])
                st_loaded[0] += 1

        def next_slab(nm_expect, si_expect):
            i = st_used[0]
            assert stream[i] == (nm_expect, si_expect), (stream[i], nm_expect, si_expect)
            st_used[0] += 1
            return i % RING

        def release():
            pump()

        def rms_stats(src_regions, src_ap_fn, nchunks, scale, rstd_slot, sq_base=SQ0):
            for c in range(nchunks):
                S.op("act", [src_regions[c]], [PL(sq_base + c)], lambda e, c=c: e.activation(
                    out=pool[:, sq_base + c, :], in_=src_ap_fn(c), func=AF.Square))
            S.op("pe", [PL(sq_base + c) for c in range(nchunks)] + ["ones_s"], [PS(ST_BANK)],
                 lambda e: [e.matmul(out=ps[ST_BANK][:], lhsT=ones_s[:], rhs=pool[:, sq_base + c, :],
                                     start=(c == 0), stop=(c == nchunks - 1)) for c in range(nchunks)][-1])
            S.op("act", [PS(ST_BANK)], [("rowb", rstd_slot)], lambda e: e.activation(
                out=rowb[:, rstd_slot, :], in_=ps[ST_BANK][:], func=AF.Sqrt, scale=scale, bias=EPS))
            S.op("dve", [("rowb", rstd_slot)], [("rowb", rstd_slot)], lambda e: e.reciprocal(
                out=rowb[:, rstd_slot, :], in_=rowb[:, rstd_slot, :]))

        hstat = {"pending": None, "n": 0}

        def _hstat_mm(m):
            n = hstat["n"]
            S.op("pe", [PL(SQ0 + m), "ones_s"], [PS(ST_BANK)], lambda e, m=m, n=n: e.matmul(
                out=ps[ST_BANK][:], lhsT=ones_s[:], rhs=pool[:, SQ0 + m, :], start=(n == 0), stop=(n == NCH - 1)))
            hstat["n"] = n + 1

        def h_updated(m):
            S.op("act", [Hk(m)], [PL(SQ0 + m)], lambda e, m=m, hm=hv(m): e.activation(
                out=pool[:, SQ0 + m, :], in_=hm, func=AF.Square))
            if hstat["pending"] is not None:
                _hstat_mm(hstat["pending"])
            hstat["pending"] = m

        def h_stats_finish(rstd_slot):
            _hstat_mm(hstat["pending"])
            assert hstat["n"] == NCH
            hstat["pending"] = None
            hstat["n"] = 0
            S.op("act", [PS(ST_BANK)], [("rowb", rstd_slot)], lambda e: e.activation(
                out=rowb[:, rstd_slot, :], in_=ps[ST_BANK][:], func=AF.Sqrt, scale=1.0, bias=EPS))
            S.op("dve", [("rowb", rstd_slot)], [("rowb", rstd_slot)], lambda e: e.reciprocal(
                out=rowb[:, rstd_slot, :], in_=rowb[:, rstd_slot, :]))

        def rmsnorm_h_to_u(gname):
            h_stats_finish(0)
            for c in range(NCH):
                S.op("dve", [Hk(c), ("rowb", 0)] + VECS, [PL(U0 + c)], lambda e, c=c, hm=hv(c): e.scalar_tensor_tensor(
                    out=pool[:, U0 + c, :], in0=hm, scalar=vcol(gname, c), in1=rowb[:, 0, :],
                    op0=ALU.mult, op1=ALU.mult))

        def proj_fm(slot, col_off, kcs, rhs_fn, rhs_regions, bank, kstride, stream_first=False):
            if stream_first:
                kl = list(kcs)
                for i, kc in enumerate(kl):
                    S.op("pe", [("ring", slot), rhs_regions[i]], [PS(bank)], lambda e, i=i, kc=kc: e.matmul(
                        out=ps[bank][:], lhsT=ring[:, slot, kc * kstride + col_off: kc * kstride + col_off + 128],
                        rhs=rhs_fn(kc), start=(i == 0), stop=(i == len(kl) - 1)))
                return

            def emit(e):
                ins = None
                for i, kc in enumerate(kcs):
                    ins = e.matmul(out=ps[bank][:], lhsT=ring[:, slot, kc * kstride + col_off: kc * kstride + col_off + 128],
                                   rhs=rhs_fn(kc), start=(i == 0), stop=(i == len(kcs) - 1))
                return ins
            S.op("pe", [("ring", slot)] + rhs_regions, [PS(bank)], emit)

        UREG = [PL(U0 + c) for c in range(NCH)]

        def ffn(win, wout, gname, after_norm=None):
            rmsnorm_h_to_u(gname)
            if after_norm is not None:
                after_norm()
            for j in range(11):
                slot = next_slab(win, j)
                for s in range(2):
                    hc = 2 * j + s
                    bg, bu = mmbank(), mmbank()
                    proj_fm(slot, s * 128, range(8), lambda kc: pool[:, U0 + kc, :], UREG, bg, 512, stream_first=(hc == 0))
                    proj_fm(slot, 256 + s * 128, range(8), lambda kc: pool[:, U0 + kc, :], UREG, bu, 512)
                    S.op("act", [PS(bg)], [PL(SQ0 + (hc % 8))], lambda e, bg=bg, hc=hc: e.activation(
                        out=pool[:, SQ0 + (hc % 8), :], in_=ps[bg][:], func=AF.Silu))
                    S.op("dve", [PS(bu), PL(SQ0 + (hc % 8))], [PL(HID0 + hc)], lambda e, bu=bu, hc=hc: e.tensor_tensor(
                        out=pool[:, HID0 + hc, :], in0=ps[bu][:], in1=pool[:, SQ0 + (hc % 8), :], op=ALU.mult))
                release()
            for cg in range(4):
                s0 = next_slab(wout, 2 * cg)
                s1 = next_slab(wout, 2 * cg + 1)
                for s in range(2):
                    m = 2 * cg + s
                    b = mmbank()

                    def emit(e, s0=s0, s1=s1, s=s, b=b):
                        ins = None
                        for half, slot in ((0, s0), (1, s1)):
                            for kc in range(11):
                                hc = half * 11 + kc
                                ins = e.matmul(out=ps[b][:], lhsT=ring[:, slot, kc * 256 + s * 128: kc * 256 + s * 128 + 128],
                                               rhs=pool[:, HID0 + hc, :], start=(hc == 0), stop=(hc == 21))
                        return ins
                    S.op("pe", [("ring", s0), ("ring", s1)] + [PL(HID0 + i) for i in range(NHC)], [PS(b)], emit)
                    S.op("dve", [PS(b), Hk(m)], [Hk(m)], lambda e, b=b, m=m, hm=hv(m): e.scalar_tensor_tensor(
                        out=hm, in0=ps[b][:], scalar=0.5, in1=hm, op0=ALU.mult, op1=ALU.add))
                    h_updated(m)
                release()

        def dump(k):
            if dbg:
                S.dma("pool", "dbg", ALLH_(), [], lambda e, k=k, hp=HP[0]: e.dma_start(out=dbg_d[k], in_=hh[:, hp * NCH:(hp + 1) * NCH, :]))

        out_evs = []
        x_t = x_d.rearrange("(n blk p) f -> n p blk f", p=128, blk=4)
        p_t = p_d.rearrange("(n blk p) f -> n p blk f", p=128, blk=4)
        y_t = y_d.rearrange("(n blk p) f -> n p blk f", p=128, blk=4)

        def load_x(t):
            S.dma("sp", "xin", [], ["xin"], lambda e, t=t: e.dma_start(out=xin[:], in_=x_t[t]))
            S.dma("sp", "pin", [], ["pin"], lambda e, t=t: e.dma_start(out=pin[:], in_=p_t[t]))

        load_x(0)
        pending_out = []
        emit_out_ref = [None]
        cast_ev = {}
        for nm in WEIGHT_NAMES:
            wv = W[nm].rearrange("(kc p) n -> p kc n", p=128)
            ev = None
            gate = []
            wi = WEIGHT_NAMES.index(nm)
            if False and wi >= 2:
                gate = [cast_ev[WEIGHT_NAMES[wi - 1]]]
            for si, (elems, pieces) in enumerate(tab[nm]):
                fine = nm in ("ffn1_w_in", "ffn1_w_out")
                for (doff, kc0, nkc, col0, ncols, dstride) in pieces:
                    src = wv[:, kc0:kc0 + nkc, col0:col0 + ncols]
                    dst = scr[nm][si, :, 0:nkc * dstride].rearrange("p (kc n) -> p kc n", n=dstride)[:, :, doff:doff + ncols]
                    semn = ("cast_%s_%d" % (nm, si)) if fine else ("cast_" + nm)
                    ev = S.dma("pool", semn, [], [], (lambda e, src=src, dst=dst: e.dma_start(out=dst, in_=src)), extra=gate)
                    gate = []
                if fine:
                    cast_ev[(nm, si)] = ev
            cast_ev[nm] = ev

        pump()

        def emit_out(t, blks=(0, 1, 2, 3), store=True):
            hp = t % 2
            hst = hh[:, hp * NCH:(hp + 1) * NCH, :].rearrange("p c t -> p (c t)").rearrange("p (blk f) -> p blk f", blk=4)
            HR = [("h", hp, i) for i in range(NCH)]
            for blk in blks:
                for fh in range(2):
                    b = mmbank()
                    S.op("pe", ALLFT + ["identf"], [PS(b)], lambda e, blk=blk, fh=fh, b=b: [e.transpose(
                        out=ps[b][:, j * 128:(j + 1) * 128], in_=ft[:, fh * 4 + j, blk * 128:(blk + 1) * 128],
                        identity=identf[:]) for j in range(4)][-1])
                    if fh:
                        S.op("act", [PS(b)], HR, lambda e, blk=blk, fh=fh, b=b, hst=hst: e.activation(
                            out=hst[:, blk, fh * 512:(fh + 1) * 512], in_=ps[b][:], func=AF.Copy))
                    else:
                        S.op("dve", [PS(b)], HR, lambda e, blk=blk, fh=fh, b=b, hst=hst: e.tensor_copy(
                            out=hst[:, blk, fh * 512:(fh + 1) * 512], in_=ps[b][:]))
            if store:
                ev = S.dma("sp", "yout%d" % hp, HR, [], lambda e, t=t, hst=hst: e.dma_start(out=y_t[t], in_=hst))
                out_evs.append(ev)

        emit_out_ref[0] = emit_out

        for t in range(NT):
            HP[0] = t % 2
            for fc in range(NCH):
                b = mmbank()
                S.op("pe", ["xin", "identf"], [PS(b)], lambda e, fc=fc, b=b: [e.transpose(
                    out=ps[b][:, blk * 128:(blk + 1) * 128], in_=xin[:, blk, fc * 128:(fc + 1) * 128],
                    identity=identf[:]) for blk in range(4)][-1])
                eng = "act" if fc % 2 else "dve"
                if eng == "act":
                    S.op("act", [PS(b)], [Hk(fc)], lambda e, fc=fc, b=b, hm=hv(fc): e.activation(out=hm, in_=ps[b][:], func=AF.Copy))
                else:
                    S.op("dve", [PS(b)], [Hk(fc)], lambda e, fc=fc, b=b, hm=hv(fc): e.tensor_copy(out=hm, in_=ps[b][:]))
                h_updated(fc)
            for pc in range(2):
                b = mmbank()
                S.op("pe", ["pin", "identf"], [PS(b)], lambda e, pc=pc, b=b: [e.transpose(
                    out=ps[b][:, blk * 128:(blk + 1) * 128], in_=pin[:, blk, pc * 128:(pc + 1) * 128],
                    identity=identf[:]) for blk in range(4)][-1])
                S.op("dve", [PS(b)], [("pT", pc)], lambda e, pc=pc, b=b: e.tensor_copy(out=pT[:, pc, :], in_=ps[b][:]))
            if t > 0 and DEFER_OUT:
                emit_out_ref[0](t - 1, blks=(0, 1), store=False)
            if t + 1 < NT:
                load_x(t + 1)
            if t == 0:
                dump(0)

            ffn("ffn1_w_in", "ffn1_w_out", "ffn1_norm",
                after_norm=(lambda t=t: emit_out_ref[0](t - 1, blks=(2, 3), store=True)) if (t > 0 and DEFER_OUT) else None)
            if t == 0:
                dump(1)

            rmsnorm_h_to_u("mix_norm")
            for half in range(2):
                sa = next_slab("w_mix_in", MIX_ORDER.index("ca%d" % half))
                sbb = next_slab("w_mix_in", MIX_ORDER.index("cb%d" % half))
                for s in range(4):
                    cc = half * 4 + s
                    ba, bb_ = mmbank(), mmbank()
                    proj_fm(sa, s * 128, range(8), lambda kc: pool[:, U0 + kc, :], UREG, ba, 512, stream_first=(cc == 0))
                    proj_fm(sbb, s * 128, range(8), lambda kc: pool[:, U0 + kc, :], UREG, bb_, 512)
                    S.op("act", [PS(bb_)], [("rowb", 2 + (cc % 2))], lambda e, bb_=bb_, cc=cc: e.activation(
                        out=rowb[:, 2 + (cc % 2), :], in_=ps[bb_][:], func=AF.Tanh, scale=0.5))
                    S.op("dve", [PS(ba), ("rowb", 2 + (cc % 2))], [("ycv", cc)], lambda e, ba=ba, cc=cc: e.scalar_tensor_tensor(
                        out=ycv[:, cc, HALO:HALO + TT], in0=rowb[:, 2 + (cc % 2), :], scalar=1.0, in1=ps[ba][:],
                        op0=ALU.add, op1=ALU.mult))
                release()
            for cc in range(NCH):
                dset = cc % 2
                dgc = [PL(DG0 + 8 * dset + i) for i in range(8)]
                dgv = pool[:, DG0 + 8 * dset: DG0 + 8 * dset + 8, :].rearrange("p c t -> p (c t)")
                S.op("pool" if t > 0 else "dve", ["identb"] + VECS, dgc, lambda e, cc=cc, dgv=dgv: e.tensor_tensor(
                    out=dgv[:, 0:CW * 128].rearrange("p (k j) -> p k j", j=128),
                    in0=identb[:].unsqueeze(1).broadcast_to([128, CW, 128]),
                    in1=vecT[:, cc, 0:CW].unsqueeze(2).broadcast_to([128, CW, 128]), op=ALU.mult))
                bc = mmbank()

                def emit_cv(e, cc=cc, dgv=dgv, bc=bc):
                    ins = None
                    for k in range(CW):
                        ins = e.matmul(out=ps[bc][:], lhsT=dgv[:, k * 128:(k + 1) * 128], rhs=ycv[:, cc, k:k + TT],
                                       start=(k == 0), stop=(k == CW - 1))
                    return ins
                S.op("pe", dgc + [("ycv", cc), ("ycvh", cc)], [PS(bc)], emit_cv)
                S.op("dve", [PS(bc)] + VECS, [FT(cc)], lambda e, cc=cc, bc=bc: e.tensor_scalar(
                    out=ft[:, cc, :], in0=ps[bc][:], scalar1=0.5, scalar2=vcol("conv_dw_b", cc), op0=ALU.mult, op1=ALU.add))
                S.op("act", [FT(cc)], [PL(YACT0 + cc)], lambda e, cc=cc: e.activation(
                    out=pool[:, YACT0 + cc, :], in_=ft[:, cc, :], func=AF.Copy))
                S.op("act", [FT(cc)], [PL(SQ0 + cc)], lambda e, cc=cc: e.activation(
                    out=pool[:, SQ0 + cc, :], in_=ft[:, cc, :], func=AF.Square))
            for cc in range(NCH):
                if t > 0:
                    S.op("pool", [("ycv", cc)], [("ycvh", cc)], lambda e, cc=cc: e.tensor_copy(
                        out=ycv[:, cc, 0:HALO], in_=ycv[:, cc, TT:TT + HALO]))
                else:
                    S.op("act", [("ycv", cc)], [("ycvh", cc)], lambda e, cc=cc: e.activation(
                        out=ycv[:, cc, 0:HALO], in_=ycv[:, cc, TT:TT + HALO], func=AF.Copy))
            bm = mmbank()
            S.op("pe", [PL(YACT0 + c) for c in range(NCH)] + ["ones_s"], [PS(bm)],
                 lambda e, bm=bm: [e.matmul(out=ps[bm][:], lhsT=ones_s[:], rhs=pool[:, YACT0 + c, :],
                                     start=(c == 0), stop=(c == NCH - 1)) for c in range(NCH)][-1])
            S.op("pe", [PL(SQ0 + c) for c in range(NCH)] + ["ones_s"], [PS(ST_BANK)],
                 lambda e: [e.matmul(out=ps[ST_BANK][:], lhsT=ones_s[:], rhs=pool[:, SQ0 + c, :],
                                     start=(c == 0), stop=(c == NCH - 1)) for c in range(NCH)][-1])
            S.op("act", [PS(bm)], [("rowb", 1)], lambda e, bm=bm: e.activation(out=rowb[:, 1, :], in_=ps[bm][:], func=AF.Copy))
            S.op("dve", [("rowb", 1)], [("rowb", 2)], lambda e: e.tensor_tensor(
                out=rowb[:, 2, :], in0=rowb[:, 1, :], in1=rowb[:, 1, :], op=ALU.mult))
            S.op("dve", [PS(ST_BANK), ("rowb", 2)], [("rowb", 0)], lambda e: e.tensor_tensor(
                out=rowb[:, 0, :], in0=ps[ST_BANK][:], in1=rowb[:, 2, :], op=ALU.subtract))
            S.op("act", [("rowb", 0)], [("rowb", 0)], lambda e: e.activation(
                out=rowb[:, 0, :], in_=rowb[:, 0, :], func=AF.Sqrt, bias=EPS))
            S.op("dve", [("rowb", 0)], [("rowb", 0)], lambda e: e.reciprocal(out=rowb[:, 0, :], in_=rowb[:, 0, :]))
            def v_proj(half):
                s_v = next_slab("w_mix_in", MIX_ORDER.index("v%d" % half))
                for bk_ in range(4):
                    b = mmbank()

                    def emit_v(e, bk_=bk_, b=b, s_v=s_v):
                        ins = None
                        for kc in range(8):
                            ins = e.matmul(out=ps[b][:], lhsT=pool[:, U0 + kc, bk_ * 128:(bk_ + 1) * 128],
                                           rhs=ring[:, s_v, kc * 512:(kc + 1) * 512], start=(kc == 0), stop=(kc == 7))
                        return ins
                    S.op("pe", [("ring", s_v)] + UREG, [PS(b)], emit_v)
                    if bk_ % 2:
                        S.op("act", [PS(b)], [("vtok", bk_, half)], lambda e, bk_=bk_, b=b, half=half: e.activation(
                            out=vtok[:, bk_, half * 512:(half + 1) * 512], in_=ps[b][:], func=AF.Copy))
                    else:
                        S.op("dve", [PS(b)], [("vtok", bk_, half)], lambda e, bk_=bk_, b=b, half=half: e.tensor_copy(
                            out=vtok[:, bk_, half * 512:(half + 1) * 512], in_=ps[b][:]))
                release()

            for hvx in range(2):
                v_proj(hvx)
                for cc in range(4 * hvx, 4 * hvx + 4):
                    S.op("dve", [FT(cc), ("rowb", 1)], [FT(cc)], lambda e, cc=cc: e.tensor_tensor(
                        out=ft[:, cc, :], in0=ft[:, cc, :], in1=rowb[:, 1, :], op=ALU.subtract))
                for cc in range(4 * hvx, 4 * hvx + 4):
                    S.op("dve", [FT(cc), ("rowb", 0)], [FT(cc)], lambda e, cc=cc: e.tensor_tensor(
                        out=ft[:, cc, :], in0=ft[:, cc, :], in1=rowb[:, 0, :], op=ALU.mult))
                    S.op("act", [FT(cc)] + VECS, [PL(YACT0 + cc)], lambda e, cc=cc: e.activation(
                        out=pool[:, YACT0 + cc, :], in_=ft[:, cc, :], func=AF.Silu,
                        scale=vcol("conv_ln_g", cc), bias=vcol("conv_ln_b", cc)))
            YACT = [PL(YACT0 + c) for c in range(NCH)]
            for half in range(2):
                sg = next_slab("w_mix_in", MIX_ORDER.index("ga%d" % half))
                sp_ = next_slab("conv_w_pw", half)
                for s in range(4):
                    m = half * 4 + s
                    bg, by = mmbank(), mmbank()
                    proj_fm(sg, s * 128, range(8), lambda kc: pool[:, U0 + kc, :], UREG, bg, 512)
                    proj_fm(sp_, s * 128, range(8), lambda kc: pool[:, YACT0 + kc, :], YACT, by, 512)
                    S.op("act", [PS(bg)], [("rowb", 2 + (m % 2))], lambda e, bg=bg, m=m: e.activation(
                        out=rowb[:, 2 + (m % 2), :], in_=ps[bg][:], func=AF.Tanh, scale=0.5))
                    S.op("dve", [PS(by), ("rowb", 2 + (m % 2))], [PL(MRG0 + m)], lambda e, by=by, m=m: e.scalar_tensor_tensor(
                        out=pool[:, MRG0 + m, :], in0=rowb[:, 2 + (m % 2), :], scalar=1.0, in1=ps[by][:],
                        op0=ALU.add, op1=ALU.mult))
                release()

            s_lr = next_slab("w_mix_in", MIX_ORDER.index("lr"))
            b = mmbank()

            def emit_lr(e, s_lr=s_lr, b=b):
                ins = None
                for kc in range(8):
                    ins = e.matmul(out=ps[b][0:16, :], lhsT=ring[:, s_lr, kc * 16:(kc + 1) * 16], rhs=pool[:, U0 + kc, :],
                                   start=(kc == 0), stop=(kc == 7))
                return ins
            S.op("pe", [("ring", s_lr)] + UREG, [PS(b)], emit_lr)
            S.op("dve", [PS(b)], ["lrT"], lambda e, b=b: e.tensor_copy(out=lrT[0:16, :], in_=ps[b][0:16, :]))
            release()
            s_q = next_slab("w_mix_in", MIX_ORDER.index("q"))
            s_k = next_slab("w_mix_in", MIX_ORDER.index("k"))
            for hd in range(4):
                b = mmbank()
                S.op("pe", ["wal_a", "wal_b", "lrT"], [PS(b)], lambda e, hd=hd, b=b: e.matmul(
                    out=ps[b][:], lhsT=wal[0:17, hd * 128:(hd + 1) * 128], rhs=lrT[0:17, :], start=True, stop=True))
                S.op("act", [PS(b)], [("rowb", 0)], lambda e, b=b: e.activation(out=rowb[:, 0, :], in_=ps[b][:], func=AF.Exp, scale=-1.0))
                S.op("act", [("rowb", 0)], [("rowb", 0)], lambda e: e.activation(out=rowb[:, 0, :], in_=rowb[:, 0, :], func=AF.Ln, bias=1.0))
                S.op("dve", [("rowb", 0), "rmask"], [("rowb", 1)], lambda e: e.tensor_tensor_scan(
                    out=rowb[:, 1, :], data0=rmask[:], data1=rowb[:, 0, :], initial=0.0, op0=ALU.mult, op1=ALU.add))
                e1 = 2 + (hd % 2)
                S.op("act", [("rowb", 1)], [("rowb", e1)], lambda e, e1=e1: e.activation(
                    out=rowb[:, e1, :], in_=rowb[:, 1, :], func=AF.Exp, scale=-1.0 / 16.0))
                S.op("act", [("rowb", 1)], ["gl4"], lambda e: e.activation(
                    out=gl4[:], in_=rowb[:, 1, :], func=AF.Exp, scale=1.0 / 16.0))
                bq, bk = mmbank(), mmbank()
                proj_fm(s_q, hd * 128, range(8), lambda kc: pool[:, U0 + kc, :], UREG, bq, 512)
                proj_fm(s_k, hd * 128, range(8), lambda kc: pool[:, U0 + kc, :], UREG, bk, 512)
                S.op("dve", [PS(bq), ("rowb", e1)], [PL(QD0 + hd)], lambda e, bq=bq, hd=hd, e1=e1: e.scalar_tensor_tensor(
                    out=pool[:, QD0 + hd, :], in0=ps[bq][:], scalar=128.0 ** -0.5, in1=rowb[:, e1, :], op0=ALU.mult, op1=ALU.mult))
                S.op("dve", [PS(bk), "gl4"], [PL(KI0 + hd)], lambda e, bk=bk, hd=hd: e.tensor_tensor(
                    out=pool[:, KI0 + hd, :], in0=ps[bk][:], in1=gl4[:], op=ALU.mult))
                S.op("dve", [("rowb", e1)], [("alast", hd)], lambda e, hd=hd, e1=e1: e.tensor_copy(
                    out=alast[:, hd, 0:4], in_=rowb[:, e1, :].rearrange("p (c k) -> p c k", k=128)[:, :, 127]))
            for bk_ in range(4):
                b = mmbank()
                S.op("pe", ["wal_a", "wal_b", "lrT"], [PS(b)], lambda e, bk_=bk_, b=b: e.matmul(
                    out=ps[b][:], lhsT=lrT[0:17, bk_ * 128:(bk_ + 1) * 128], rhs=wal[0:17, :], start=True, stop=True))
                S.op("act", [PS(b)], [("rowb", 0)], lambda e, b=b: e.activation(out=rowb[:, 0, :], in_=ps[b][:], func=AF.Exp, scale=-1.0))
                S.op("act", [("rowb", 0)], [("rowb", 0)], lambda e: e.activation(out=rowb[:, 0, :], in_=rowb[:, 0, :], func=AF.Ln, bias=1.0))
                b2 = mmbank()
                S.op("pe", [("rowb", 0), "triU"], [PS(b2)], lambda e, b2=b2: e.matmul(
                    out=ps[b2][:], lhsT=triU[:], rhs=rowb[:, 0, :], start=True, stop=True))
                S.op("act", [PS(b2)], [("rowb", 1)], lambda e, b2=b2: e.activation(
                    out=rowb[:, 1, :], in_=ps[b2][:], func=AF.Exp, scale=-1.0 / 16.0))
                b3 = mmbank()

                def emit_kt(e, bk_=bk_, b3=b3, s_k=s_k):
                    ins = None
                    for kc in range(8):
                        ins = e.matmul(out=ps[b3][:], lhsT=pool[:, U0 + kc, bk_ * 128:(bk_ + 1) * 128],
                                       rhs=ring[:, s_k, kc * 512:(kc + 1) * 512], start=(kc == 0), stop=(kc == 7))
                    return ins
                S.op("pe", [("ring", s_k)] + UREG, [PS(b3)], emit_kt)
                S.op("dve", [PS(b3), ("rowb", 1)], [("kdtok", bk_)], lambda e, bk_=bk_, b3=b3: e.tensor_tensor(
                    out=kdtok[:, bk_, :], in0=ps[b3][:], in1=rowb[:, 1, :], op=ALU.mult))
            release()
            release()
            for half in range(2):
                s_g = next_slab("w_mix_in", MIX_ORDER.index("g%d" % half))
                for s in range(4):
                    m = half * 4 + s
                    b = mmbank()
                    proj_fm(s_g, s * 128, range(8), lambda kc: pool[:, U0 + kc, :], UREG, b, 512)
                    S.op("act", [PS(b)], [PL(GS0 + m)], lambda e, b=b, m=m: e.activation(
                        out=pool[:, GS0 + m, :], in_=ps[b][:], func=AF.Silu))
                release()
            SCB = [2, 3]
            DLB = [0, 1]
            OBK = {0: (4, 5), 1: (6, 7)}

            def obank(bk_, hd):
                return OBK[bk_ % 2][hd // 2]

            def ocol(hd, ec):
                return (hd % 2) * 256 + ec * 128

            def stage_a(bk_):
                tsl = slice(bk_ * 128, (bk_ + 1) * 128)
                for bb in OBK[bk_ % 2]:
                    S.op("pe", ["zerob", PL(U0)], [PS(bb)], lambda e, bb=bb: e.matmul(
                        out=ps[bb][:], lhsT=zerob[:], rhs=pool[:, U0, :], start=True, stop=False, skip_group_check=True))
                for hd in range(4):
                    sb_ = SCB[hd % 2]
                    S.op("pe", [PL(KI0 + hd), PL(QD0 + hd)], [PS(sb_)], lambda e, hd=hd, tsl=tsl, sb_=sb_: e.matmul(
                        out=ps[sb_][:, 0:128], lhsT=pool[:, KI0 + hd, tsl], rhs=pool[:, QD0 + hd, tsl], start=True, stop=True))
                    S.op("dve", [PS(sb_), "maskT"], [("scm", hd)], lambda e, hd=hd, sb_=sb_: e.tensor_tensor(
                        out=scm[:, hd, :], in0=ps[sb_][:, 0:128], in1=maskT[:], op=ALU.mult))
                for hd in range(4):
                    ob_ = obank(bk_, hd)

                    def emit_i(e, hd=hd, bk_=bk_, tsl=tsl, ob_=ob_):
                        ins = None
                        for ec in range(2):
                            ins = e.matmul(out=ps[ob_][:, ocol(hd, ec):ocol(hd, ec) + 128],
                                           lhsT=vtok[:, bk_, hd * 256 + ec * 128: hd * 256 + ec * 128 + 128],
                                           rhs=scm[:, hd, :], start=False, stop=False, skip_group_check=True)
                        return ins
                    S.op("pe", [("scm", hd)] + [("vtok", bk_, i) for i in range(2)], [PS(ob_)], emit_i)

            def stage_b(bk_):
                par = bk_ % 2
                tsl = slice(bk_ * 128, (bk_ + 1) * 128)
                for hd in range(4):
                    ob_ = obank(bk_, hd)

                    def emit_x(e, hd=hd, ob_=ob_, tsl=tsl, par=par):
                        ins = None
                        for ec in range(2):
                            o0 = ocol(hd, ec)
                            ins = e.matmul(out=ps[ob_][:, o0:o0 + 128], lhsT=Sb[:, hd * 2 + par, ec * 128:(ec + 1) * 128],
                                           rhs=pool[:, QD0 + hd, tsl], start=False, stop=True, skip_group_check=True)
                        return ins
                    S.op("pe", [("Sb", hd * 2 + par), PL(QD0 + hd)], [PS(ob_)], emit_x)
                for hd in range(4):
                    db_ = DLB[hd % 2]
                    S.op("pe", [("kdtok", bk_)] + [("vtok", bk_, i) for i in range(2)], [PS(db_)],
                         lambda e, hd=hd, bk_=bk_, db_=db_: e.matmul(
                             out=ps[db_][:, 0:256], lhsT=kdtok[:, bk_, hd * 128:(hd + 1) * 128],
                             rhs=vtok[:, bk_, hd * 256:(hd + 1) * 256], start=True, stop=True))
                    S.op("dve", [PS(db_), ("Sst", hd), ("alast", hd)], [("Sst", hd)],
                         lambda e, hd=hd, bk_=bk_, db_=db_: e.scalar_tensor_tensor(
                             out=Sst[:, hd, :], in0=Sst[:, hd, :], scalar=alast[:, hd, bk_:bk_ + 1], in1=ps[db_][:, 0:256],
                             op0=ALU.mult, op1=ALU.add))
                    if hd % 2 and t > 0:
                        S.op("pool", [("Sst", hd)], [("Sb", hd * 2 + (1 - par))], lambda e, hd=hd, par=par: e.tensor_copy(
                            out=Sb[:, hd * 2 + (1 - par), :], in_=Sst[:, hd, :]))
                    else:
                        S.op("act", [("Sst", hd)], [("Sb", hd * 2 + (1 - par))], lambda e, hd=hd, par=par: e.activation(
                            out=Sb[:, hd * 2 + (1 - par), :], in_=Sst[:, hd, :], func=AF.Copy))

            def evac(bk_):
                for i, bb in enumerate(OBK[bk_ % 2]):
                    S.op("act", [PS(bb)], [FT(4 * i + j) for j in range(4)], lambda e, i=i, bb=bb, bk_=bk_: e.activation(
                        out=ft[:, 4 * i:4 * i + 4, bk_ * 128:(bk_ + 1) * 128],
                        in_=ps[bb][:].rearrange("p (m t) -> p m t", m=4), func=AF.Copy))

            stage_a(0)
            for bk_ in range(4):
                if bk_ + 1 < 4:
                    stage_a(bk_ + 1)
                stage_b(bk_)
                evac(bk_)
            TG0 = 32

            def gb_half(half):
                sg = next_slab("w_mix_in", MIX_ORDER.index("gb%d" % half))
                for s_ in range(4):
                    m = half * 4 + s_
                    bg = mmbank()
                    proj_fm(sg, s_ * 128, range(8), lambda kc: pool[:, U0 + kc, :], UREG, bg, 512)
                    S.op("act", [PS(bg)], [PL(TG0 + m)], lambda e, bg=bg, m=m: e.activation(
                        out=pool[:, TG0 + m, :], in_=ps[bg][:], func=AF.Tanh, scale=0.5))
                release()

            for m in range(NCH):
                S.op("act", [FT(m)], [PL(SQ0 + m)], lambda e, m=m: e.activation(
                    out=pool[:, SQ0 + m, :], in_=ft[:, m, :], func=AF.Square))
            gb_half(0)
            NB_ = [2, 3, 0, 1]
            for hd in range(4):
                nb_ = NB_[hd]
                S.op("pe", [PL(SQ0 + 2 * hd), PL(SQ0 + 2 * hd + 1), "ones_s"], [PS(nb_)], lambda e, hd=hd, nb_=nb_: [e.matmul(
                    out=ps[nb_][:], lhsT=ones_s[:], rhs=pool[:, SQ0 + 2 * hd + ec, :], start=(ec == 0), stop=(ec == 1)) for ec in range(2)][-1])
            for hd in range(4):
                nb_ = NB_[hd]
                S.op("act", [PS(nb_)], [("rowb", hd)], lambda e, hd=hd, nb_=nb_: e.activation(
                    out=rowb[:, hd, :], in_=ps[nb_][:], func=AF.Sqrt, scale=4.0, bias=EPS))
            for hd in range(4):
                S.op("dve", [("rowb", hd)], [("rowb", hd)], lambda e, hd=hd: e.reciprocal(out=rowb[:, hd, :], in_=rowb[:, hd, :]))
            gb_half(1)
            for hd in range(4):
                for ec in range(2):
                    m = hd * 2 + ec
                    S.op("dve", [FT(m), ("rowb", hd)] + VECS, [FT(m)], lambda e, m=m, hd=hd: e.scalar_tensor_tensor(
                        out=ft[:, m, :], in0=ft[:, m, :], scalar=vcol("gla_norm", m), in1=rowb[:, hd, :],
                        op0=ALU.mult, op1=ALU.mult))
                for ec in range(2):
                    m = hd * 2 + ec
                    S.op("pool" if t > 0 else "dve", [FT(m), PL(GS0 + m)], [PL(OG0 + m)], lambda e, m=m: e.tensor_tensor(
                        out=pool[:, OG0 + m, :], in0=ft[:, m, :], in1=pool[:, GS0 + m, :], op=ALU.mult))
            OG = [PL(OG0 + c) for c in range(NCH)]
            for half in range(2):
                so = next_slab("gla_w_o", half)
                for s_ in range(4):
                    m = half * 4 + s_
                    by = mmbank()
                    proj_fm(so, s_ * 128, range(8), lambda kc: pool[:, OG0 + kc, :], OG, by, 512)
                    S.op("dve", [PS(by), PL(TG0 + m)], [("rowb", m % 4)], lambda e, by=by, m=m: e.scalar_tensor_tensor(
                        out=rowb[:, m % 4, :], in0=pool[:, TG0 + m, :], scalar=1.0, in1=ps[by][:],
                        op0=ALU.add, op1=ALU.mult))
                    S.op("pool" if t > 0 else "dve", [("rowb", m % 4), PL(MRG0 + m)], [PL(MRG0 + m)], lambda e, m=m: e.tensor_tensor(
                        out=pool[:, MRG0 + m, :], in0=rowb[:, m % 4, :], in1=pool[:, MRG0 + m, :], op=ALU.add))
                release()
            MRG = [PL(MRG0 + c) for c in range(NCH)]
            for half in range(2):
                so = next_slab("w_mix_out", half)
                for s in range(4):
                    m = half * 4 + s
                    b = mmbank()
                    proj_fm(so, s * 128, range(8), lambda kc: pool[:, MRG0 + kc, :], MRG, b, 512)
                    S.op("dve", [PS(b), Hk(m)], [Hk(m)], lambda e, b=b, m=m, hm=hv(m): e.scalar_tensor_tensor(
                        out=hm, in0=ps[b][:], scalar=0.5, in1=hm, op0=ALU.mult, op1=ALU.add))
                    h_updated(m)
                release()
            if t == 0:
                dump(2)

            ffn("ffn2_w_in", "ffn2_w_out", "ffn2_norm")
            if t == 0:
                dump(3)

            spp = next_slab("ple_w_proj", 0)
            PT = [("pT", 0), ("pT", 1)]
            for m in range(NCH):
                bp = mmbank()
                proj_fm(spp, m * 128, range(2), lambda kc: pT[:, kc, :], PT, bp, 1024)
                S.op("act", [PS(bp)], [FT(m)], lambda e, bp=bp, m=m: e.activation(out=ft[:, m, :], in_=ps[bp][:], func=AF.Copy))
            release()
            rmsnorm_h_to_u("ple_norm")
            sg0 = next_slab("ple_w_gate", 0)
            sg1 = next_slab("ple_w_gate", 1)
            for m in range(NCH):
                sg = sg0 if m < 4 else sg1
                s_ = m % 4
                bg = mmbank()
                proj_fm(sg, s_ * 128, range(8), lambda kc: pool[:, U0 + kc, :], UREG, bg, 512, stream_first=(m == 0))
                S.op("act", [PS(bg)], [("rowb", 2 + (m % 2))], lambda e, bg=bg, m=m: e.activation(
                    out=rowb[:, 2 + (m % 2), :], in_=ps[bg][:], func=AF.Tanh, scale=0.5))
                S.op("dve", [FT(m), ("rowb", 2 + (m % 2))], [FT(m)], lambda e, m=m: e.scalar_tensor_tensor(
                    out=ft[:, m, :], in0=rowb[:, 2 + (m % 2), :], scalar=1.0, in1=ft[:, m, :], op0=ALU.add, op1=ALU.mult))
            release()
            release()
            rms_stats(ALLFT, lambda c: ft[:, c, :], NCH, 0.25, 1)
            for m in range(NCH):
                S.op("pool" if t > 0 else "dve", [FT(m), ("rowb", 1)], [FT(m)], lambda e, m=m: e.tensor_tensor(
                    out=ft[:, m, :], in0=ft[:, m, :], in1=rowb[:, 1, :], op=ALU.mult))
                S.op("dve", [FT(m), Hk(m)] + VECS, [Hk(m)], lambda e, m=m, hm=hv(m): e.scalar_tensor_tensor(
                    out=hm, in0=ft[:, m, :], scalar=vecT[:, m, 63:64], in1=hm, op0=ALU.mult, op1=ALU.add))
                h_updated(m)
            if t == 0:
                dump(4)

            h_stats_finish(0)
            for c in range(NCH):
                S.op("dve", [Hk(c), ("rowb", 0)] + VECS, [FT(c)], lambda e, c=c, hm=hv(c): e.scalar_tensor_tensor(
                    out=ft[:, c, :], in0=hm, scalar=vcol("final_norm", c), in1=rowb[:, 0, :],
                    op0=ALU.mult, op1=ALU.mult))
            pending_out.append(t)
            if not DEFER_OUT:
                emit_out_ref[0](t)

        if DEFER_OUT:
            emit_out(NT - 1)
        S.final_wait("sp", out_evs)
        assert st_used[0] == len(stream)
        S.emit_all(block)
    return nc


_W_KEYS = ["ffn1_w_in", "ffn1_w_out", "w_mix_in", "conv_w_pw", "gla_w_o", "w_mix_out",
           "ffn2_w_in", "ffn2_w_out", "ple_w_gate", "ple_w_proj"]
_V_KEYS = ["ffn1_norm", "mix_norm", "conv_dw_b", "conv_ln_g", "conv_ln_b", "gla_norm",
           "ffn2_norm", "ple_norm", "ple_post_norm"]


def make_in_maps(inputs, n_cores, T):
    f = lambda a: np.ascontiguousarray(np.asarray(a, dtype=np.float32))
    shared = {}
    for k in _W_KEYS:
        shared[k] = f(inputs[k])[0]
    for k in _V_KEYS:
        shared[k] = f(inputs[k]).reshape(1, D)
    shared["final_norm"] = f(inputs["final_norm"]).reshape(1, D)
    shared["conv_dw_w"] = f(inputs["conv_dw_w"])[0]
    shared["gla_w_alpha"] = f(inputs["gla_w_alpha"])[0]
    shared["gla_b_alpha"] = f(inputs["gla_b_alpha"]).reshape(1, 512)
    x = f(inputs["x"])
    p = f(inputs["p"])[0]
    maps = []
    for c in range(n_cores):
        m = dict(shared)
        m["x"] = np.ascontiguousarray(x[c, :T])
        m["p"] = np.ascontiguousarray(p[c, :T])
        maps.append(m)
    return maps


def kernel(**inputs):
    B, T, _ = inputs["x"].shape
    nc = build_program(T)
    in_maps = make_in_maps(inputs, B, T)
    res = run_bass_kernel_spmd(nc, in_maps, core_ids=list(range(B)))
    out = np.stack([np.asarray(r["y"], dtype=np.float32) for r in res.results], axis=0)
    return out
```

```python
import contextlib
import numpy as np
import concourse.bass as bass
import concourse.mybir as mybir
from concourse.bass_utils import run_bass_kernel_spmd

F32 = mybir.dt.float32
BF16 = mybir.dt.bfloat16
AF = mybir.ActivationFunctionType
ALU = mybir.AluOpType

D = 1024
DFF = 2816
NIN = 7184
PLE = 256
CW = 31
HALO = CW - 1
TT = 512
EPS = 1e-6
NCH = D // 128
NHC = DFF // 128
RING = 5
SLOT = 4096
NPOOL = 48
DEFER_OUT = True
STRICT_SAME_ENGINE = True

ENG_NAMES = ("pe", "act", "dve", "pool", "sp")


class Sched:
    def __init__(self, nc, stack):
        self.nc = nc
        self.stack = stack
        self.prog = {e: [] for e in ENG_NAMES}
        self.count = {e: 0 for e in ENG_NAMES}
        self.known = {e: {} for e in ENG_NAMES}
        self.sems = {}
        self.dma_count = {}
        self.last_write = {}
        self.readers = {}
        for e in ENG_NAMES:
            self.sem(e)

    def sem(self, name):
        if name not in self.sems:
            self.sems[name] = self.stack.enter_context(self.nc.semaphore("s_" + name))
        return self.sems[name]

    def _deps(self, reads, writes, extra):
        deps = set(extra)
        for r in reads:
            if r in self.last_write:
                deps.add(self.last_write[r])
            if isinstance(r, tuple) and r[0] == "ps":
                for ev in self.readers.get(r, ()):
                    deps.add(ev)
        for w in writes:
            if w in self.last_write:
                deps.add(self.last_write[w])
            for ev in self.readers.get(w, ()):
                deps.add(ev)
        return deps

    def _waits(self, eng, deps):
        need = {}
        for (k, v) in deps:
            if k == eng:
                if eng in ("pe", "sp"):
                    continue
                if (not STRICT_SAME_ENGINE) and eng != "pool" and v < self.count[eng]:
                    continue
            if self.known[eng].get(k, 0) >= v:
                continue
            need[k] = max(need.get(k, 0), v)
        for k, v in need.items():
            self.known[eng][k] = v
        return [(self.sems[k], v) for k, v in need.items()]

    def _record(self, ev, reads, writes):
        for w in writes:
            self.last_write[w] = ev
            self.readers[w] = []
        for r in reads:
            if r not in writes:
                self.readers.setdefault(r, []).append(ev)

    def op(self, eng, reads, writes, emit, extra=()):
        reads = list(reads)
        writes = list(writes)
        waits = self._waits(eng, self._deps(reads, writes, extra))
        self.count[eng] += 1
        ev = (eng, self.count[eng])
        sem = self.sems[eng]

        def run(e, waits=waits, emit=emit, sem=sem):
            for s, v in waits:
                e.wait_ge(s, v)
            ins = emit(e)
            ins.then_inc(sem, 1)

        self.prog[eng].append(run)
        self._record(ev, reads, writes)
        return ev

    def dma(self, eng, semname, reads, writes, emit, extra=()):
        reads = list(reads)
        writes = list(writes)
        self.sem(semname)
        waits = self._waits(eng, self._deps(reads, writes, extra))
        self.dma_count[semname] = self.dma_count.get(semname, 0) + 1
        ev = (semname, 16 * self.dma_count[semname])
        sem = self.sems[semname]

        def run(e, waits=waits, emit=emit, sem=sem):
            for s, v in waits:
                e.wait_ge(s, v)
            ins = emit(e)
            ins.then_inc(sem, 16)

        self.prog[eng].append(run)
        self._record(ev, reads, writes)
        return ev

    def final_wait(self, eng, events):
        waits = self._waits(eng, set(events))

        def run(e, waits=waits):
            for s, v in waits:
                e.wait_ge(s, v)

        self.prog[eng].append(run)

    def emit_all(self, block):
        progs = self.prog

        @block.tensor
        def _(e):
            for f in progs["pe"]:
                f(e)

        @block.scalar
        def _(e):
            for f in progs["act"]:
                f(e)

        @block.vector
        def _(e):
            for f in progs["dve"]:
                f(e)

        @block.gpsimd
        def _(e):
            for f in progs["pool"]:
                f(e)

        @block.sync
        def _(e):
            for f in progs["sp"]:
                f(e)


MIX_SLABS = {
    "ca0": (0, 512), "ca1": (512, 512), "cb0": (1024, 512), "cb1": (1536, 512),
    "q": (2048, 512), "k": (2560, 512), "v0": (3072, 512), "v1": (3584, 512),
    "g0": (4096, 512), "g1": (4608, 512), "lr": (5120, 16),
    "ga0": (5136, 512), "ga1": (5648, 512), "gb0": (6160, 512), "gb1": (6672, 512),
}
MIX_ORDER = list(MIX_SLABS.keys())

WEIGHT_NAMES = ["ffn1_w_in", "ffn1_w_out", "w_mix_in", "conv_w_pw", "gla_w_o", "w_mix_out",
                "ffn2_w_in", "ffn2_w_out", "ple_w_gate", "ple_w_proj"]


def slab_table():
    tab = {}
    for nm in ("ffn1_w_in", "ffn2_w_in"):
        sl = []
        for j in range(11):
            pieces = []
            for g in range(2):
                pieces.append((g * 256, 0, 8, g * DFF + j * 256, 256, 512))
            sl.append((8 * 512, pieces))
        tab[nm] = sl
    for nm in ("ffn1_w_out", "ffn2_w_out"):
        sl = []
        for s in range(8):
            cg, half = s // 2, s % 2
            sl.append((11 * 256, [(0, half * 11, 11, cg * 256, 256, 256)]))
        tab[nm] = sl
    sl = []
    for nm in MIX_ORDER:
        c0, ncol = MIX_SLABS[nm]
        sl.append((8 * ncol, [(0, 0, 8, c0, ncol, ncol)]))
    tab["w_mix_in"] = sl
    for nm in ("conv_w_pw", "gla_w_o", "w_mix_out", "ple_w_gate"):
        tab[nm] = [(8 * 512, [(0, 0, 8, s * 512, 512, 512)]) for s in range(2)]
    tab["ple_w_proj"] = [(2 * 1024, [(0, 0, 2, 0, 1024, 1024)])]
    return tab


def build_program(T, dbg=False):
    NT = T // TT
    nc = bass.Bass("TRN2", target_bir_lowering=False)
    tab = slab_table()

    def din(name, shape):
        return nc.dram_tensor(name, list(shape), F32, kind="ExternalInput").ap()

    x_d = din("x", (T, D))
    p_d = din("p", (T, PLE))
    W = {}
    W["ffn1_w_in"] = din("ffn1_w_in", (D, 2 * DFF))
    W["ffn1_w_out"] = din("ffn1_w_out", (DFF, D))
    W["w_mix_in"] = din("w_mix_in", (D, NIN))
    W["conv_w_pw"] = din("conv_w_pw", (D, D))
    W["gla_w_o"] = din("gla_w_o", (D, D))
    W["w_mix_out"] = din("w_mix_out", (D, D))
    W["ffn2_w_in"] = din("ffn2_w_in", (D, 2 * DFF))
    W["ffn2_w_out"] = din("ffn2_w_out", (DFF, D))
    W["ple_w_gate"] = din("ple_w_gate", (D, D))
    W["ple_w_proj"] = din("ple_w_proj", (PLE, D))
    VEC_NAMES = ["ffn1_norm", "mix_norm", "conv_dw_b", "conv_ln_g", "conv_ln_b", "gla_norm",
                 "ffn2_norm", "ple_norm", "ple_post_norm", "final_norm"]
    V = {n: din(n, (1, D)) for n in VEC_NAMES}
    dww_d = din("conv_dw_w", (CW, D))
    walpha_d = din("gla_w_alpha", (16, 512))
    balpha_d = din("gla_b_alpha", (1, 512))
    y_d = nc.dram_tensor("y", [T, D], F32, kind="ExternalOutput").ap()
    dbg_d = None
    if dbg:
        dbg_d = nc.dram_tensor("dbg", [8, 128, NCH, TT], F32, kind="ExternalOutput").ap()

    scr = {}
    for nm in WEIGHT_NAMES:
        nsl = len(tab[nm])
        scr[nm] = nc.dram_tensor("scr_" + nm, [nsl, 128, SLOT], BF16, kind="Internal").ap()

    with contextlib.ExitStack() as st:
        def sb(name, shape, dt):
            return st.enter_context(nc.sbuf_tensor(name, list(shape), dt))

        hh = sb("hh", (128, 2 * NCH, TT), F32)
        xin = sb("xin", (128, 4, D), F32)
        pin = sb("pin", (128, 4, PLE), F32)
        ft = sb("ft", (128, NCH, TT), F32)
        pool = sb("pool", (128, NPOOL, TT), BF16)
        ycv = sb("ycv", (128, NCH, HALO + TT), BF16)
        vtok = sb("vtok", (128, 4, D), BF16)
        kdtok = sb("kdtok", (128, 4, 512), BF16)
        ring = sb("ring", (128, RING, SLOT), BF16)
        rowb = sb("rowb", (128, 4, TT), F32)
        gl4 = sb("gl4", (128, TT), F32)
        Sst = sb("Sst", (128, 4, 256), F32)
        Sb = sb("Sb", (128, 8, 256), BF16)
        scm = sb("scm", (128, 4, 128), BF16)
        zerob = sb("zerob", (128, 128), BF16)
        identf = sb("identf", (128, 128), F32)
        onesf = sb("onesf", (128, 128), F32)
        identb = sb("identb", (128, 128), BF16)
        ones_s = sb("ones_s", (128, 128), BF16)
        maskT = sb("maskT", (128, 128), F32)
        triU = sb("triU", (128, 128), F32)
        rmask = sb("rmask", (128, TT), F32)
        vecT = sb("vecT", (128, NCH, 64), F32)
        wal = sb("wal", (32, 512), BF16)
        lrT = sb("lrT", (32, TT), BF16)
        pT = sb("pT", (128, 2, TT), BF16)
        alast = sb("alast", (128, 4, 8), F32)
        ps = [st.enter_context(nc.psum_tensor("ps%d" % i, [128, 512], F32)) for i in range(8)]

        S = Sched(nc, st)
        block = st.enter_context(nc.Block())

        mm_banks = [0, 1, 4, 5, 6, 7]
        mm_ctr = [0]

        def mmbank():
            b = mm_banks[mm_ctr[0] % len(mm_banks)]
            mm_ctr[0] += 1
            return b

        ST_BANK = 2
        SC_BANK = 3

        def PS(b):
            return ("ps", b)

        def PL(i):
            return ("pool", i)

        HP = [0]

        def Hk(i):
            return ("h", HP[0], i)

        def hv(c):
            return hh[:, HP[0] * NCH + c, :]

        def FT(i):
            return ("ft", i)

        def ALLH_():
            return [Hk(i) for i in range(NCH)]
        ALLFT = [FT(i) for i in range(NCH)]

        U0, SQ0, X0 = 0, 8, 16
        HID0 = 16
        YACT0, MRG0 = 16, 24
        DG0 = 32
        QD0, KI0, GS0, OG0 = 32, 36, 16, 40

        VC = {n: 32 + i for i, n in enumerate(VEC_NAMES)}

        def vcol(name, c):
            return vecT[:, c, VC[name]:VC[name] + 1]

        S.op("pool", [], ["identf"], lambda e: e.memset(identf[:], 0.0))
        S.op("pool", ["identf"], ["identf"], lambda e: e.affine_select(
            out=identf[:], in_=identf[:], pattern=[[-1, 128]], compare_op=ALU.not_equal, fill=1.0,
            base=0, channel_multiplier=1))
        S.op("pool", [], ["onesf"], lambda e: e.memset(onesf[:], 1.0))
        S.op("pool", [], ["zerob"], lambda e: e.memset(zerob[:], 0.0))
        S.op("pool", ["identf"], ["identb"], lambda e: e.tensor_copy(out=identb[:], in_=identf[:]))
        S.op("pool", [], ["ones_s"], lambda e: e.memset(ones_s[:], 1.0 / 1024.0))

        S.op("pool", ["onesf"], ["maskT"], lambda e: e.affine_select(
            out=maskT[:], in_=onesf[:], pattern=[[1, 128]], compare_op=ALU.is_ge, fill=0.0, base=0, channel_multiplier=-1))
        S.op("pool", ["onesf"], ["triU"], lambda e: e.affine_select(
            out=triU[:], in_=onesf[:], pattern=[[-1, 128]], compare_op=ALU.is_gt, fill=0.0, base=0, channel_multiplier=1))

        S.op("pool", [], ["rmask"], lambda e: e.memset(rmask[:], 1.0))
        S.op("pool", ["rmask"], ["rmask"], lambda e: e.memset(rmask[:].rearrange("p (c k) -> p c k", k=128)[:, :, 0:1], 0.0))
        S.op("pool", [], ["lrT"], lambda e: e.memset(lrT[:], 1.0))
        S.op("pool", [], ["Sst"], lambda e: e.memset(Sst[:], 0.0))
        S.op("pool", [], [("Sb", i) for i in range(8)], lambda e: e.memset(Sb[:], 0.0))
        S.op("pool", [], [("ycvh", c) for c in range(NCH)], lambda e: e.memset(ycv[:, :, 0:HALO], 0.0))
        xin_zero_ev = S.op("pool", [], ["xin"], lambda e: e.memset(xin[:, 0, :], 0.0))

        def ld_vecs(e):
            ins = e.dma_start(out=xin[0:CW, 0, :], in_=dww_d)
            return ins

        S.dma("pool", "cst_dw", [], ["xin_dw"], ld_vecs, extra=[xin_zero_ev])
        for i, n in enumerate(VEC_NAMES):
            S.dma("pool", "cst_v%d" % i, [], ["xin_v%d" % i],
                  (lambda e, i=i, n=n: e.dma_start(out=xin[32 + i:33 + i, 0, :], in_=V[n])), extra=[xin_zero_ev])
        S.dma("pool", "cst_wa", [], ["wal_a"], lambda e: e.dma_start(out=wal[0:16, :], in_=walpha_d))
        S.dma("pool", "cst_wb", [], ["wal_b"], lambda e: e.dma_start(out=wal[16:17, :], in_=balpha_d))

        for c in range(NCH):
            b = mmbank()
            S.op("pe", ["xin", "xin_dw", "identf"] + ["xin_v%d" % i for i in range(len(VEC_NAMES))], [PS(b)], lambda e, c=c, b=b: e.transpose(
                out=ps[b][:, 0:128], in_=xin[:, 0, c * 128:(c + 1) * 128], identity=identf[:]))
            S.op("dve", [PS(b)], [("vecT", c)], lambda e, c=c, b=b: e.tensor_copy(
                out=vecT[:, c, :], in_=ps[b][:, 0:64]))
            S.op("dve", [("vecT", c)], [("vecT", c)], lambda e, c=c: e.tensor_scalar(
                out=vecT[:, c, 63:64], in0=vecT[:, c, VC["ple_post_norm"]:VC["ple_post_norm"] + 1], scalar1=0.5, scalar2=None,
                op0=ALU.mult))
        VECS = [("vecT", c) for c in range(NCH)]

        stream = []
        for t in range(NT):
            for nm in ("ffn1_w_in", "ffn1_w_out"):
                for si in range(len(tab[nm])):
                    stream.append((nm, si))
            mo = {n: i for i, n in enumerate(MIX_ORDER)}
            for key in ("ca0", "cb0", "ca1", "cb1", "v0", "v1"):
                stream.append(("w_mix_in", mo[key]))
            stream += [("w_mix_in", mo["ga0"]), ("conv_w_pw", 0), ("w_mix_in", mo["ga1"]), ("conv_w_pw", 1)]
            for key in ("lr", "q", "k", "g0", "g1"):
                stream.append(("w_mix_in", mo[key]))
            stream += [("w_mix_in", mo["gb0"]), ("w_mix_in", mo["gb1"]), ("gla_w_o", 0), ("gla_w_o", 1)]
            stream += [("w_mix_out", 0), ("w_mix_out", 1)]
            for nm in ("ffn2_w_in", "ffn2_w_out"):
                for si in range(len(tab[nm])):
                    stream.append((nm, si))
            stream += [("ple_w_proj", 0), ("ple_w_gate", 0), ("ple_w_gate", 1)]
        st_loaded = [0]
        st_used = [0]

        def pump():
            while st_loaded[0] < len(stream) and st_loaded[0] < st_used[0] + RING:
                i = st_loaded[0]
                nm, si = stream[i]
                slot = i % RING
                elems = tab[nm][si][0]
                S.dma("sp", "ring%d" % slot, [], [("ring", slot)],
                      (lambda e, nm=nm, si=si, slot=slot, elems=elems: e.dma_start(
                          out=ring[:, slot, 0:elems], in_=scr[nm][si, :, 0:elems])),
                      extra=[cast_ev[(nm, si)]])
                st_loaded[0] += 1

        def next_slab(nm_expect, si_expect):
            i = st_used[0]
            assert stream[i] == (nm_expect, si_expect), (stream[i], nm_expect, si_expect)
            st_used[0] += 1
            return i % RING

        def release():
            pump()

        def rms_stats(src_regions, src_ap_fn, nchunks, scale, rstd_slot, sq_base=SQ0):
            for c in range(nchunks):
                S.op("act", [src_regions[c]], [PL(sq_base + c)], lambda e, c=c: e.activation(
                    out=pool[:, sq_base + c, :], in_=src_ap_fn(c), func=AF.Square))
            S.op("pe", [PL(sq_base + c) for c in range(nchunks)] + ["ones_s"], [PS(ST_BANK)],
                 lambda e: [e.matmul(out=ps[ST_BANK][:], lhsT=ones_s[:], rhs=pool[:, sq_base + c, :],
                                     start=(c == 0), stop=(c == nchunks - 1)) for c in range(nchunks)][-1])
            S.op("act", [PS(ST_BANK)], [("rowb", rstd_slot)], lambda e: e.activation(
                out=rowb[:, rstd_slot, :], in_=ps[ST_BANK][:], func=AF.Sqrt, scale=scale, bias=EPS))
            S.op("dve", [("rowb", rstd_slot)], [("rowb", rstd_slot)], lambda e: e.reciprocal(
                out=rowb[:, rstd_slot, :], in_=rowb[:, rstd_slot, :]))

        hstat = {"pending": None, "n": 0}

        def _hstat_mm(m):
            n = hstat["n"]
            S.op("pe", [PL(SQ0 + m), "ones_s"], [PS(ST_BANK)], lambda e, m=m, n=n: e.matmul(
                out=ps[ST_BANK][:], lhsT=ones_s[:], rhs=pool[:, SQ0 + m, :], start=(n == 0), stop=(n == NCH - 1)))
            hstat["n"] = n + 1

        def h_updated(m):
            S.op("act", [Hk(m)], [PL(SQ0 + m)], lambda e, m=m, hm=hv(m): e.activation(
                out=pool[:, SQ0 + m, :], in_=hm, func=AF.Square))
            if hstat["pending"] is not None:
                _hstat_mm(hstat["pending"])
            hstat["pending"] = m

        def h_stats_finish(rstd_slot):
            _hstat_mm(hstat["pending"])
            assert hstat["n"] == NCH
            hstat["pending"] = None
            hstat["n"] = 0
            S.op("act", [PS(ST_BANK)], [("rowb", rstd_slot)], lambda e: e.activation(
                out=rowb[:, rstd_slot, :], in_=ps[ST_BANK][:], func=AF.Sqrt, scale=1.0, bias=EPS))
            S.op("dve", [("rowb", rstd_slot)], [("rowb", rstd_slot)], lambda e: e.reciprocal(
                out=rowb[:, rstd_slot, :], in_=rowb[:, rstd_slot, :]))

        def rmsnorm_h_to_u(gname):
            h_stats_finish(0)
            for c in range(NCH):
                S.op("dve", [Hk(c), ("rowb", 0)] + VECS, [PL(U0 + c)], lambda e, c=c, hm=hv(c): e.scalar_tensor_tensor(
                    out=pool[:, U0 + c, :], in0=hm, scalar=vcol(gname, c), in1=rowb[:, 0, :],
                    op0=ALU.mult, op1=ALU.mult))

        def proj_fm(slot, col_off, kcs, rhs_fn, rhs_regions, bank, kstride, stream_first=False):
            if stream_first:
                kl = list(kcs)
                for i, kc in enumerate(kl):
                    S.op("pe", [("ring", slot), rhs_regions[i]], [PS(bank)], lambda e, i=i, kc=kc: e.matmul(
                        out=ps[bank][:], lhsT=ring[:, slot, kc * kstride + col_off: kc * kstride + col_off + 128],
                        rhs=rhs_fn(kc), start=(i == 0), stop=(i == len(kl) - 1)))
                return

            def emit(e):
                ins = None
                for i, kc in enumerate(kcs):
                    ins = e.matmul(out=ps[bank][:], lhsT=ring[:, slot, kc * kstride + col_off: kc * kstride + col_off + 128],
                                   rhs=rhs_fn(kc), start=(i == 0), stop=(i == len(kcs) - 1))
                return ins
            S.op("pe", [("ring", slot)] + rhs_regions, [PS(bank)], emit)

        UREG = [PL(U0 + c) for c in range(NCH)]

        def ffn(win, wout, gname, after_norm=None):
            rmsnorm_h_to_u(gname)
            if after_norm is not None:
                after_norm()
            for j in range(11):
                slot = next_slab(win, j)
                for s in range(2):
                    hc = 2 * j + s
                    bg, bu = mmbank(), mmbank()
                    proj_fm(slot, s * 128, range(8), lambda kc: pool[:, U0 + kc, :], UREG, bg, 512, stream_first=(hc == 0))
                    proj_fm(slot, 256 + s * 128, range(8), lambda kc: pool[:, U0 + kc, :], UREG, bu, 512)
                    S.op("act", [PS(bg)], [PL(SQ0 + (hc % 8))], lambda e, bg=bg, hc=hc: e.activation(
                        out=pool[:, SQ0 + (hc % 8), :], in_=ps[bg][:], func=AF.Silu))
                    S.op("dve", [PS(bu), PL(SQ0 + (hc % 8))], [PL(HID0 + hc)], lambda e, bu=bu, hc=hc: e.tensor_tensor(
                        out=pool[:, HID0 + hc, :], in0=ps[bu][:], in1=pool[:, SQ0 + (hc % 8), :], op=ALU.mult))
                release()
            for cg in range(4):
                s0 = next_slab(wout, 2 * cg)
                s1 = next_slab(wout, 2 * cg + 1)
                for s in range(2):
                    m = 2 * cg + s
                    b = mmbank()

                    def emit(e, s0=s0, s1=s1, s=s, b=b):
                        ins = None
                        for half, slot in ((0, s0), (1, s1)):
                            for kc in range(11):
                                hc = half * 11 + kc
                                ins = e.matmul(out=ps[b][:], lhsT=ring[:, slot, kc * 256 + s * 128: kc * 256 + s * 128 + 128],
                                               rhs=pool[:, HID0 + hc, :], start=(hc == 0), stop=(hc == 21))
                        return ins
                    S.op("pe", [("ring", s0), ("ring", s1)] + [PL(HID0 + i) for i in range(NHC)], [PS(b)], emit)
                    S.op("dve", [PS(b), Hk(m)], [Hk(m)], lambda e, b=b, m=m, hm=hv(m): e.scalar_tensor_tensor(
                        out=hm, in0=ps[b][:], scalar=0.5, in1=hm, op0=ALU.mult, op1=ALU.add))
                    h_updated(m)
                release()

        def dump(k):
            if dbg:
                S.dma("pool", "dbg", ALLH_(), [], lambda e, k=k, hp=HP[0]: e.dma_start(out=dbg_d[k], in_=hh[:, hp * NCH:(hp + 1) * NCH, :]))

        out_evs = []
        x_t = x_d.rearrange("(n blk p) f -> n p blk f", p=128, blk=4)
        p_t = p_d.rearrange("(n blk p) f -> n p blk f", p=128, blk=4)
        y_t = y_d.rearrange("(n blk p) f -> n p blk f", p=128, blk=4)

        def load_x(t):
            S.dma("sp", "xin", [], ["xin"], lambda e, t=t: e.dma_start(out=xin[:], in_=x_t[t]))
            S.dma("sp", "pin", [], ["pin"], lambda e, t=t: e.dma_start(out=pin[:], in_=p_t[t]))

        load_x(0)
        pending_out = []
        emit_out_ref = [None]
        cast_ev = {}
        n_per_tile = len(stream) // NT
        for (nm, si) in stream[:n_per_tile]:
            wv = W[nm].rearrange("(kc p) n -> p kc n", p=128)
            elems, pieces = tab[nm][si]
            ev = None
            for (doff, kc0, nkc, col0, ncols, dstride) in pieces:
                src = wv[:, kc0:kc0 + nkc, col0:col0 + ncols]
                dst = scr[nm][si, :, 0:nkc * dstride].rearrange("p (kc n) -> p kc n", n=dstride)[:, :, doff:doff + ncols]
                ev = S.dma("pool", "cast_%s_%d" % (nm, si), [], [], (lambda e, src=src, dst=dst: e.dma_start(out=dst, in_=src)))
            cast_ev[(nm, si)] = ev
        assert len(cast_ev) == sum(len(v) for v in tab.values())

        pump()

        def emit_out(t, blks=(0, 1, 2, 3), store=True):
            hp = t % 2
            hst = hh[:, hp * NCH:(hp + 1) * NCH, :].rearrange("p c t -> p (c t)").rearrange("p (blk f) -> p blk f", blk=4)
            HR = [("h", hp, i) for i in range(NCH)]
            for blk in blks:
                for fh in range(2):
                    b = mmbank()
                    S.op("pe", ALLFT + ["identf"], [PS(b)], lambda e, blk=blk, fh=fh, b=b: [e.transpose(
                        out=ps[b][:, j * 128:(j + 1) * 128], in_=ft[:, fh * 4 + j, blk * 128:(blk + 1) * 128],
                        identity=identf[:]) for j in range(4)][-1])
                    if fh:
                        S.op("act", [PS(b)], HR, lambda e, blk=blk, fh=fh, b=b, hst=hst: e.activation(
                            out=hst[:, blk, fh * 512:(fh + 1) * 512], in_=ps[b][:], func=AF.Copy))
                    else:
                        S.op("dve", [PS(b)], HR, lambda e, blk=blk, fh=fh, b=b, hst=hst: e.tensor_copy(
                            out=hst[:, blk, fh * 512:(fh + 1) * 512], in_=ps[b][:]))
            if store:
                ev = S.dma("sp", "yout%d" % hp, HR, [], lambda e, t=t, hst=hst: e.dma_start(out=y_t[t], in_=hst))
                out_evs.append(ev)

        emit_out_ref[0] = emit_out

        for t in range(NT):
            HP[0] = t % 2
            for fc in range(NCH):
                b = mmbank()
                S.op("pe", ["xin", "identf"], [PS(b)], lambda e, fc=fc, b=b: [e.transpose(
                    out=ps[b][:, blk * 128:(blk + 1) * 128], in_=xin[:, blk, fc * 128:(fc + 1) * 128],
                    identity=identf[:]) for blk in range(4)][-1])
                eng = "act" if fc % 2 else "dve"
                if eng == "act":
                    S.op("act", [PS(b)], [Hk(fc)], lambda e, fc=fc, b=b, hm=hv(fc): e.activation(out=hm, in_=ps[b][:], func=AF.Copy))
                else:
                    S.op("dve", [PS(b)], [Hk(fc)], lambda e, fc=fc, b=b, hm=hv(fc): e.tensor_copy(out=hm, in_=ps[b][:]))
                h_updated(fc)
            for pc in range(2):
                b = mmbank()
                S.op("pe", ["pin", "identf"], [PS(b)], lambda e, pc=pc, b=b: [e.transpose(
                    out=ps[b][:, blk * 128:(blk + 1) * 128], in_=pin[:, blk, pc * 128:(pc + 1) * 128],
                    identity=identf[:]) for blk in range(4)][-1])
                S.op("dve", [PS(b)], [("pT", pc)], lambda e, pc=pc, b=b: e.tensor_copy(out=pT[:, pc, :], in_=ps[b][:]))
            if t > 0 and DEFER_OUT:
                emit_out_ref[0](t - 1, blks=(0, 1), store=False)
            if t + 1 < NT:
                load_x(t + 1)
            if t == 0:
                dump(0)

            ffn("ffn1_w_in", "ffn1_w_out", "ffn1_norm",
                after_norm=(lambda t=t: emit_out_ref[0](t - 1, blks=(2, 3), store=True)) if (t > 0 and DEFER_OUT) else None)
            if t == 0:
                dump(1)

            rmsnorm_h_to_u("mix_norm")
            for half in range(2):
                sa = next_slab("w_mix_in", MIX_ORDER.index("ca%d" % half))
                sbb = next_slab("w_mix_in", MIX_ORDER.index("cb%d" % half))
                for s in range(4):
                    cc = half * 4 + s
                    ba, bb_ = mmbank(), mmbank()
                    proj_fm(sa, s * 128, range(8), lambda kc: pool[:, U0 + kc, :], UREG, ba, 512, stream_first=(cc == 0))
                    proj_fm(sbb, s * 128, range(8), lambda kc: pool[:, U0 + kc, :], UREG, bb_, 512)
                    S.op("act", [PS(bb_)], [("rowb", 2 + (cc % 2))], lambda e, bb_=bb_, cc=cc: e.activation(
                        out=rowb[:, 2 + (cc % 2), :], in_=ps[bb_][:], func=AF.Tanh, scale=0.5))
                    S.op("dve", [PS(ba), ("rowb", 2 + (cc % 2))], [("ycv", cc)], lambda e, ba=ba, cc=cc: e.scalar_tensor_tensor(
                        out=ycv[:, cc, HALO:HALO + TT], in0=rowb[:, 2 + (cc % 2), :], scalar=1.0, in1=ps[ba][:],
                        op0=ALU.add, op1=ALU.mult))
                release()
            for cc in range(NCH):
                dset = cc % 2
                dgc = [PL(DG0 + 8 * dset + i) for i in range(8)]
                dgv = pool[:, DG0 + 8 * dset: DG0 + 8 * dset + 8, :].rearrange("p c t -> p (c t)")
                S.op("pool" if t > 0 else "dve", ["identb"] + VECS, dgc, lambda e, cc=cc, dgv=dgv: e.tensor_tensor(
                    out=dgv[:, 0:CW * 128].rearrange("p (k j) -> p k j", j=128),
                    in0=identb[:].unsqueeze(1).broadcast_to([128, CW, 128]),
                    in1=vecT[:, cc, 0:CW].unsqueeze(2).broadcast_to([128, CW, 128]), op=ALU.mult))
                bc = mmbank()

                def emit_cv(e, cc=cc, dgv=dgv, bc=bc):
                    ins = None
                    for k in range(CW):
                        ins = e.matmul(out=ps[bc][:], lhsT=dgv[:, k * 128:(k + 1) * 128], rhs=ycv[:, cc, k:k + TT],
                                       start=(k == 0), stop=(k == CW - 1))
                    return ins
                S.op("pe", dgc + [("ycv", cc), ("ycvh", cc)], [PS(bc)], emit_cv)
                S.op("dve", [PS(bc)] + VECS, [FT(cc)], lambda e, cc=cc, bc=bc: e.tensor_scalar(
                    out=ft[:, cc, :], in0=ps[bc][:], scalar1=0.5, scalar2=vcol("conv_dw_b", cc), op0=ALU.mult, op1=ALU.add))
                S.op("act", [FT(cc)], [PL(YACT0 + cc)], lambda e, cc=cc: e.activation(
                    out=pool[:, YACT0 + cc, :], in_=ft[:, cc, :], func=AF.Copy))
                S.op("act", [FT(cc)], [PL(SQ0 + cc)], lambda e, cc=cc: e.activation(
                    out=pool[:, SQ0 + cc, :], in_=ft[:, cc, :], func=AF.Square))
            for cc in range(NCH):
                if t > 0:
                    S.op("pool", [("ycv", cc)], [("ycvh", cc)], lambda e, cc=cc: e.tensor_copy(
                        out=ycv[:, cc, 0:HALO], in_=ycv[:, cc, TT:TT + HALO]))
                else:
                    S.op("act", [("ycv", cc)], [("ycvh", cc)], lambda e, cc=cc: e.activation(
                        out=ycv[:, cc, 0:HALO], in_=ycv[:, cc, TT:TT + HALO], func=AF.Copy))
            bm = mmbank()
            S.op("pe", [PL(YACT0 + c) for c in range(NCH)] + ["ones_s"], [PS(bm)],
                 lambda e, bm=bm: [e.matmul(out=ps[bm][:], lhsT=ones_s[:], rhs=pool[:, YACT0 + c, :],
                                     start=(c == 0), stop=(c == NCH - 1)) for c in range(NCH)][-1])
            S.op("pe", [PL(SQ0 + c) for c in range(NCH)] + ["ones_s"], [PS(ST_BANK)],
                 lambda e: [e.matmul(out=ps[ST_BANK][:], lhsT=ones_s[:], rhs=pool[:, SQ0 + c, :],
                                     start=(c == 0), stop=(c == NCH - 1)) for c in range(NCH)][-1])
            S.op("act", [PS(bm)], [("rowb", 1)], lambda e, bm=bm: e.activation(out=rowb[:, 1, :], in_=ps[bm][:], func=AF.Copy))
            S.op("dve", [("rowb", 1)], [("rowb", 2)], lambda e: e.tensor_tensor(
                out=rowb[:, 2, :], in0=rowb[:, 1, :], in1=rowb[:, 1, :], op=ALU.mult))
            S.op("dve", [PS(ST_BANK), ("rowb", 2)], [("rowb", 0)], lambda e: e.tensor_tensor(
                out=rowb[:, 0, :], in0=ps[ST_BANK][:], in1=rowb[:, 2, :], op=ALU.subtract))
            S.op("act", [("rowb", 0)], [("rowb", 0)], lambda e: e.activation(
                out=rowb[:, 0, :], in_=rowb[:, 0, :], func=AF.Sqrt, bias=EPS))
            S.op("dve", [("rowb", 0)], [("rowb", 0)], lambda e: e.reciprocal(out=rowb[:, 0, :], in_=rowb[:, 0, :]))
            def v_proj(half):
                s_v = next_slab("w_mix_in", MIX_ORDER.index("v%d" % half))
                for bk_ in range(4):
                    b = mmbank()

                    def emit_v(e, bk_=bk_, b=b, s_v=s_v):
                        ins = None
                        for kc in range(8):
                            ins = e.matmul(out=ps[b][:], lhsT=pool[:, U0 + kc, bk_ * 128:(bk_ + 1) * 128],
                                           rhs=ring[:, s_v, kc * 512:(kc + 1) * 512], start=(kc == 0), stop=(kc == 7))
                        return ins
                    S.op("pe", [("ring", s_v)] + UREG, [PS(b)], emit_v)
                    if bk_ % 2:
                        S.op("act", [PS(b)], [("vtok", bk_, half)], lambda e, bk_=bk_, b=b, half=half: e.activation(
                            out=vtok[:, bk_, half * 512:(half + 1) * 512], in_=ps[b][:], func=AF.Copy))
                    else:
                        S.op("dve", [PS(b)], [("vtok", bk_, half)], lambda e, bk_=bk_, b=b, half=half: e.tensor_copy(
                            out=vtok[:, bk_, half * 512:(half + 1) * 512], in_=ps[b][:]))
                release()

            for hvx in range(2):
                v_proj(hvx)
                for cc in range(4 * hvx, 4 * hvx + 4):
                    S.op("dve", [FT(cc), ("rowb", 1)], [FT(cc)], lambda e, cc=cc: e.tensor_tensor(
                        out=ft[:, cc, :], in0=ft[:, cc, :], in1=rowb[:, 1, :], op=ALU.subtract))
                for cc in range(4 * hvx, 4 * hvx + 4):
                    S.op("dve", [FT(cc), ("rowb", 0)], [FT(cc)], lambda e, cc=cc: e.tensor_tensor(
                        out=ft[:, cc, :], in0=ft[:, cc, :], in1=rowb[:, 0, :], op=ALU.mult))
                    S.op("act", [FT(cc)] + VECS, [PL(YACT0 + cc)], lambda e, cc=cc: e.activation(
                        out=pool[:, YACT0 + cc, :], in_=ft[:, cc, :], func=AF.Silu,
                        scale=vcol("conv_ln_g", cc), bias=vcol("conv_ln_b", cc)))
            YACT = [PL(YACT0 + c) for c in range(NCH)]
            for half in range(2):
                sg = next_slab("w_mix_in", MIX_ORDER.index("ga%d" % half))
                sp_ = next_slab("conv_w_pw", half)
                for s in range(4):
                    m = half * 4 + s
                    bg, by = mmbank(), mmbank()
                    proj_fm(sg, s * 128, range(8), lambda kc: pool[:, U0 + kc, :], UREG, bg, 512)
                    proj_fm(sp_, s * 128, range(8), lambda kc: pool[:, YACT0 + kc, :], YACT, by, 512)
                    S.op("act", [PS(bg)], [("rowb", 2 + (m % 2))], lambda e, bg=bg, m=m: e.activation(
                        out=rowb[:, 2 + (m % 2), :], in_=ps[bg][:], func=AF.Tanh, scale=0.5))
                    S.op("dve", [PS(by), ("rowb", 2 + (m % 2))], [PL(MRG0 + m)], lambda e, by=by, m=m: e.scalar_tensor_tensor(
                        out=pool[:, MRG0 + m, :], in0=rowb[:, 2 + (m % 2), :], scalar=1.0, in1=ps[by][:],
                        op0=ALU.add, op1=ALU.mult))
                release()

            s_lr = next_slab("w_mix_in", MIX_ORDER.index("lr"))
            b = mmbank()

            def emit_lr(e, s_lr=s_lr, b=b):
                ins = None
                for kc in range(8):
                    ins = e.matmul(out=ps[b][0:16, :], lhsT=ring[:, s_lr, kc * 16:(kc + 1) * 16], rhs=pool[:, U0 + kc, :],
                                   start=(kc == 0), stop=(kc == 7))
                return ins
            S.op("pe", [("ring", s_lr)] + UREG, [PS(b)], emit_lr)
            S.op("dve", [PS(b)], ["lrT"], lambda e, b=b: e.tensor_copy(out=lrT[0:16, :], in_=ps[b][0:16, :]))
            release()
            s_q = next_slab("w_mix_in", MIX_ORDER.index("q"))
            s_k = next_slab("w_mix_in", MIX_ORDER.index("k"))
            for hd in range(4):
                b = mmbank()
                S.op("pe", ["wal_a", "wal_b", "lrT"], [PS(b)], lambda e, hd=hd, b=b: e.matmul(
                    out=ps[b][:], lhsT=wal[0:17, hd * 128:(hd + 1) * 128], rhs=lrT[0:17, :], start=True, stop=True))
                S.op("act", [PS(b)], [("rowb", 0)], lambda e, b=b: e.activation(out=rowb[:, 0, :], in_=ps[b][:], func=AF.Exp, scale=-1.0))
                S.op("act", [("rowb", 0)], [("rowb", 0)], lambda e: e.activation(out=rowb[:, 0, :], in_=rowb[:, 0, :], func=AF.Ln, bias=1.0))
                S.op("dve", [("rowb", 0), "rmask"], [("rowb", 1)], lambda e: e.tensor_tensor_scan(
                    out=rowb[:, 1, :], data0=rmask[:], data1=rowb[:, 0, :], initial=0.0, op0=ALU.mult, op1=ALU.add))
                e1 = 2 + (hd % 2)
                S.op("act", [("rowb", 1)], [("rowb", e1)], lambda e, e1=e1: e.activation(
                    out=rowb[:, e1, :], in_=rowb[:, 1, :], func=AF.Exp, scale=-1.0 / 16.0))
                S.op("act", [("rowb", 1)], ["gl4"], lambda e: e.activation(
                    out=gl4[:], in_=rowb[:, 1, :], func=AF.Exp, scale=1.0 / 16.0))
                bq, bk = mmbank(), mmbank()
                proj_fm(s_q, hd * 128, range(8), lambda kc: pool[:, U0 + kc, :], UREG, bq, 512)
                proj_fm(s_k, hd * 128, range(8), lambda kc: pool[:, U0 + kc, :], UREG, bk, 512)
                S.op("dve", [PS(bq), ("rowb", e1)], [PL(QD0 + hd)], lambda e, bq=bq, hd=hd, e1=e1: e.scalar_tensor_tensor(
                    out=pool[:, QD0 + hd, :], in0=ps[bq][:], scalar=128.0 ** -0.5, in1=rowb[:, e1, :], op0=ALU.mult, op1=ALU.mult))
                S.op("dve", [PS(bk), "gl4"], [PL(KI0 + hd)], lambda e, bk=bk, hd=hd: e.tensor_tensor(
                    out=pool[:, KI0 + hd, :], in0=ps[bk][:], in1=gl4[:], op=ALU.mult))
                S.op("dve", [("rowb", e1)], [("alast", hd)], lambda e, hd=hd, e1=e1: e.tensor_copy(
                    out=alast[:, hd, 0:4], in_=rowb[:, e1, :].rearrange("p (c k) -> p c k", k=128)[:, :, 127]))
            for bk_ in range(4):
                b = mmbank()
                S.op("pe", ["wal_a", "wal_b", "lrT"], [PS(b)], lambda e, bk_=bk_, b=b: e.matmul(
                    out=ps[b][:], lhsT=lrT[0:17, bk_ * 128:(bk_ + 1) * 128], rhs=wal[0:17, :], start=True, stop=True))
                S.op("act", [PS(b)], [("rowb", 0)], lambda e, b=b: e.activation(out=rowb[:, 0, :], in_=ps[b][:], func=AF.Exp, scale=-1.0))
                S.op("act", [("rowb", 0)], [("rowb", 0)], lambda e: e.activation(out=rowb[:, 0, :], in_=rowb[:, 0, :], func=AF.Ln, bias=1.0))
                b2 = mmbank()
                S.op("pe", [("rowb", 0), "triU"], [PS(b2)], lambda e, b2=b2: e.matmul(
                    out=ps[b2][:], lhsT=triU[:], rhs=rowb[:, 0, :], start=True, stop=True))
                S.op("act", [PS(b2)], [("rowb", 1)], lambda e, b2=b2: e.activation(
                    out=rowb[:, 1, :], in_=ps[b2][:], func=AF.Exp, scale=-1.0 / 16.0))
                b3 = mmbank()

                def emit_kt(e, bk_=bk_, b3=b3, s_k=s_k):
                    ins = None
                    for kc in range(8):
                        ins = e.matmul(out=ps[b3][:], lhsT=pool[:, U0 + kc, bk_ * 128:(bk_ + 1) * 128],
                                       rhs=ring[:, s_k, kc * 512:(kc + 1) * 512], start=(kc == 0), stop=(kc == 7))
                    return ins
                S.op("pe", [("ring", s_k)] + UREG, [PS(b3)], emit_kt)
                S.op("dve", [PS(b3), ("rowb", 1)], [("kdtok", bk_)], lambda e, bk_=bk_, b3=b3: e.tensor_tensor(
                    out=kdtok[:, bk_, :], in0=ps[b3][:], in1=rowb[:, 1, :], op=ALU.mult))
            release()
            release()
            for half in range(2):
                s_g = next_slab("w_mix_in", MIX_ORDER.index("g%d" % half))
                for s in range(4):
                    m = half * 4 + s
                    b = mmbank()
                    proj_fm(s_g, s * 128, range(8), lambda kc: pool[:, U0 + kc, :], UREG, b, 512)
                    S.op("act", [PS(b)], [PL(GS0 + m)], lambda e, b=b, m=m: e.activation(
                        out=pool[:, GS0 + m, :], in_=ps[b][:], func=AF.Silu))
                release()
            SCB = [2, 3]
            DLB = [0, 1]
            OBK = {0: (4, 5), 1: (6, 7)}

            def obank(bk_, hd):
                return OBK[bk_ % 2][hd // 2]

            def ocol(hd, ec):
                return (hd % 2) * 256 + ec * 128

            def stage_a(bk_):
                tsl = slice(bk_ * 128, (bk_ + 1) * 128)
                for bb in OBK[bk_ % 2]:
                    S.op("pe", ["zerob", PL(U0)], [PS(bb)], lambda e, bb=bb: e.matmul(
                        out=ps[bb][:], lhsT=zerob[:], rhs=pool[:, U0, :], start=True, stop=False, skip_group_check=True))
                for hd in range(4):
                    sb_ = SCB[hd % 2]
                    S.op("pe", [PL(KI0 + hd), PL(QD0 + hd)], [PS(sb_)], lambda e, hd=hd, tsl=tsl, sb_=sb_: e.matmul(
                        out=ps[sb_][:, 0:128], lhsT=pool[:, KI0 + hd, tsl], rhs=pool[:, QD0 + hd, tsl], start=True, stop=True))
                    S.op("dve", [PS(sb_), "maskT"], [("scm", hd)], lambda e, hd=hd, sb_=sb_: e.tensor_tensor(
                        out=scm[:, hd, :], in0=ps[sb_][:, 0:128], in1=maskT[:], op=ALU.mult))
                for hd in range(4):
                    ob_ = obank(bk_, hd)

                    def emit_i(e, hd=hd, bk_=bk_, tsl=tsl, ob_=ob_):
                        ins = None
                        for ec in range(2):
                            ins = e.matmul(out=ps[ob_][:, ocol(hd, ec):ocol(hd, ec) + 128],
                                           lhsT=vtok[:, bk_, hd * 256 + ec * 128: hd * 256 + ec * 128 + 128],
                                           rhs=scm[:, hd, :], start=False, stop=False, skip_group_check=True)
                        return ins
                    S.op("pe", [("scm", hd)] + [("vtok", bk_, i) for i in range(2)], [PS(ob_)], emit_i)

            def stage_b(bk_):
                par = bk_ % 2
                tsl = slice(bk_ * 128, (bk_ + 1) * 128)
                for hd in range(4):
                    ob_ = obank(bk_, hd)

                    def emit_x(e, hd=hd, ob_=ob_, tsl=tsl, par=par):
                        ins = None
                        for ec in range(2):
                            o0 = ocol(hd, ec)
                            ins = e.matmul(out=ps[ob_][:, o0:o0 + 128], lhsT=Sb[:, hd * 2 + par, ec * 128:(ec + 1) * 128],
                                           rhs=pool[:, QD0 + hd, tsl], start=False, stop=True, skip_group_check=True)
                        return ins
                    S.op("pe", [("Sb", hd * 2 + par), PL(QD0 + hd)], [PS(ob_)], emit_x)
                for hd in range(4):
                    db_ = DLB[hd % 2]
                    S.op("pe", [("kdtok", bk_)] + [("vtok", bk_, i) for i in range(2)], [PS(db_)],
                         lambda e, hd=hd, bk_=bk_, db_=db_: e.matmul(
                             out=ps[db_][:, 0:256], lhsT=kdtok[:, bk_, hd * 128:(hd + 1) * 128],
                             rhs=vtok[:, bk_, hd * 256:(hd + 1) * 256], start=True, stop=True))
                    S.op("dve", [PS(db_), ("Sst", hd), ("alast", hd)], [("Sst", hd)],
                         lambda e, hd=hd, bk_=bk_, db_=db_: e.scalar_tensor_tensor(
                             out=Sst[:, hd, :], in0=Sst[:, hd, :], scalar=alast[:, hd, bk_:bk_ + 1], in1=ps[db_][:, 0:256],
                             op0=ALU.mult, op1=ALU.add))
                    if hd % 2 and t > 0:
                        S.op("pool", [("Sst", hd)], [("Sb", hd * 2 + (1 - par))], lambda e, hd=hd, par=par: e.tensor_copy(
                            out=Sb[:, hd * 2 + (1 - par), :], in_=Sst[:, hd, :]))
                    else:
                        S.op("act", [("Sst", hd)], [("Sb", hd * 2 + (1 - par))], lambda e, hd=hd, par=par: e.activation(
                            out=Sb[:, hd * 2 + (1 - par), :], in_=Sst[:, hd, :], func=AF.Copy))

            def evac(bk_):
                for i, bb in enumerate(OBK[bk_ % 2]):
                    S.op("act", [PS(bb)], [FT(4 * i + j) for j in range(4)], lambda e, i=i, bb=bb, bk_=bk_: e.activation(
                        out=ft[:, 4 * i:4 * i + 4, bk_ * 128:(bk_ + 1) * 128],
                        in_=ps[bb][:].rearrange("p (m t) -> p m t", m=4), func=AF.Copy))

            stage_a(0)
            for bk_ in range(4):
                if bk_ + 1 < 4:
                    stage_a(bk_ + 1)
                stage_b(bk_)
                evac(bk_)
            TG0 = 32

            def gb_half(half):
                sg = next_slab("w_mix_in", MIX_ORDER.index("gb%d" % half))
                for s_ in range(4):
                    m = half * 4 + s_
                    bg = mmbank()
                    proj_fm(sg, s_ * 128, range(8), lambda kc: pool[:, U0 + kc, :], UREG, bg, 512)
                    S.op("act", [PS(bg)], [PL(TG0 + m)], lambda e, bg=bg, m=m: e.activation(
                        out=pool[:, TG0 + m, :], in_=ps[bg][:], func=AF.Tanh, scale=0.5))
                release()

            for m in range(NCH):
                S.op("act", [FT(m)], [PL(SQ0 + m)], lambda e, m=m: e.activation(
                    out=pool[:, SQ0 + m, :], in_=ft[:, m, :], func=AF.Square))
            gb_half(0)
            NB_ = [2, 3, 0, 1]
            for hd in range(4):
                nb_ = NB_[hd]
                S.op("pe", [PL(SQ0 + 2 * hd), PL(SQ0 + 2 * hd + 1), "ones_s"], [PS(nb_)], lambda e, hd=hd, nb_=nb_: [e.matmul(
                    out=ps[nb_][:], lhsT=ones_s[:], rhs=pool[:, SQ0 + 2 * hd + ec, :], start=(ec == 0), stop=(ec == 1)) for ec in range(2)][-1])
            for hd in range(4):
                nb_ = NB_[hd]
                S.op("act", [PS(nb_)], [("rowb", hd)], lambda e, hd=hd, nb_=nb_: e.activation(
                    out=rowb[:, hd, :], in_=ps[nb_][:], func=AF.Sqrt, scale=4.0, bias=EPS))
            for hd in range(4):
                S.op("dve", [("rowb", hd)], [("rowb", hd)], lambda e, hd=hd: e.reciprocal(out=rowb[:, hd, :], in_=rowb[:, hd, :]))
            gb_half(1)
            for hd in range(4):
                for ec in range(2):
                    m = hd * 2 + ec
                    S.op("dve", [FT(m), ("rowb", hd)] + VECS, [FT(m)], lambda e, m=m, hd=hd: e.scalar_tensor_tensor(
                        out=ft[:, m, :], in0=ft[:, m, :], scalar=vcol("gla_norm", m), in1=rowb[:, hd, :],
                        op0=ALU.mult, op1=ALU.mult))
                for ec in range(2):
                    m = hd * 2 + ec
                    S.op("pool" if t > 0 else "dve", [FT(m), PL(GS0 + m)], [PL(OG0 + m)], lambda e, m=m: e.tensor_tensor(
                        out=pool[:, OG0 + m, :], in0=ft[:, m, :], in1=pool[:, GS0 + m, :], op=ALU.mult))
            OG = [PL(OG0 + c) for c in range(NCH)]
            for half in range(2):
                so = next_slab("gla_w_o", half)
                for s_ in range(4):
                    m = half * 4 + s_
                    by = mmbank()
                    proj_fm(so, s_ * 128, range(8), lambda kc: pool[:, OG0 + kc, :], OG, by, 512)
                    S.op("dve", [PS(by), PL(TG0 + m)], [("rowb", m % 4)], lambda e, by=by, m=m: e.scalar_tensor_tensor(
                        out=rowb[:, m % 4, :], in0=pool[:, TG0 + m, :], scalar=1.0, in1=ps[by][:],
                        op0=ALU.add, op1=ALU.mult))
                    S.op("pool" if t > 0 else "dve", [("rowb", m % 4), PL(MRG0 + m)], [PL(MRG0 + m)], lambda e, m=m: e.tensor_tensor(
                        out=pool[:, MRG0 + m, :], in0=rowb[:, m % 4, :], in1=pool[:, MRG0 + m, :], op=ALU.add))
                release()
            MRG = [PL(MRG0 + c) for c in range(NCH)]
            for half in range(2):
                so = next_slab("w_mix_out", half)
                for s in range(4):
                    m = half * 4 + s
                    b = mmbank()
                    proj_fm(so, s * 128, range(8), lambda kc: pool[:, MRG0 + kc, :], MRG, b, 512)
                    S.op("dve", [PS(b), Hk(m)], [Hk(m)], lambda e, b=b, m=m, hm=hv(m): e.scalar_tensor_tensor(
                        out=hm, in0=ps[b][:], scalar=0.5, in1=hm, op0=ALU.mult, op1=ALU.add))
                    h_updated(m)
                release()
            if t == 0:
                dump(2)

            ffn("ffn2_w_in", "ffn2_w_out", "ffn2_norm")
            if t == 0:
                dump(3)

            spp = next_slab("ple_w_proj", 0)
            PT = [("pT", 0), ("pT", 1)]
            for m in range(NCH):
                bp = mmbank()
                proj_fm(spp, m * 128, range(2), lambda kc: pT[:, kc, :], PT, bp, 1024)
                S.op("act", [PS(bp)], [FT(m)], lambda e, bp=bp, m=m: e.activation(out=ft[:, m, :], in_=ps[bp][:], func=AF.Copy))
            release()
            rmsnorm_h_to_u("ple_norm")
            sg0 = next_slab("ple_w_gate", 0)
            sg1 = next_slab("ple_w_gate", 1)
            for m in range(NCH):
                sg = sg0 if m < 4 else sg1
                s_ = m % 4
                bg = mmbank()
                proj_fm(sg, s_ * 128, range(8), lambda kc: pool[:, U0 + kc, :], UREG, bg, 512, stream_first=(m == 0))
                S.op("act", [PS(bg)], [("rowb", 2 + (m % 2))], lambda e, bg=bg, m=m: e.activation(
                    out=rowb[:, 2 + (m % 2), :], in_=ps[bg][:], func=AF.Tanh, scale=0.5))
                S.op("dve", [FT(m), ("rowb", 2 + (m % 2))], [FT(m)], lambda e, m=m: e.scalar_tensor_tensor(
                    out=ft[:, m, :], in0=rowb[:, 2 + (m % 2), :], scalar=1.0, in1=ft[:, m, :], op0=ALU.add, op1=ALU.mult))
            release()
            release()
            rms_stats(ALLFT, lambda c: ft[:, c, :], NCH, 0.25, 1)
            for m in range(NCH):
                S.op("pool" if t > 0 else "dve", [FT(m), ("rowb", 1)], [FT(m)], lambda e, m=m: e.tensor_tensor(
                    out=ft[:, m, :], in0=ft[:, m, :], in1=rowb[:, 1, :], op=ALU.mult))
                S.op("dve", [FT(m), Hk(m)] + VECS, [Hk(m)], lambda e, m=m, hm=hv(m): e.scalar_tensor_tensor(
                    out=hm, in0=ft[:, m, :], scalar=vecT[:, m, 63:64], in1=hm, op0=ALU.mult, op1=ALU.add))
                h_updated(m)
            if t == 0:
                dump(4)

            h_stats_finish(0)
            for c in range(NCH):
                S.op("dve", [Hk(c), ("rowb", 0)] + VECS, [FT(c)], lambda e, c=c, hm=hv(c): e.scalar_tensor_tensor(
                    out=ft[:, c, :], in0=hm, scalar=vcol("final_norm", c), in1=rowb[:, 0, :],
                    op0=ALU.mult, op1=ALU.mult))
            pending_out.append(t)
            if not DEFER_OUT:
                emit_out_ref[0](t)

        if DEFER_OUT:
            emit_out(NT - 1)
        S.final_wait("sp", out_evs)
        assert st_used[0] == len(stream)
        S.emit_all(block)
    return nc


_W_KEYS = ["ffn1_w_in", "ffn1_w_out", "w_mix_in", "conv_w_pw", "gla_w_o", "w_mix_out",
           "ffn2_w_in", "ffn2_w_out", "ple_w_gate", "ple_w_proj"]
_V_KEYS = ["ffn1_norm", "mix_norm", "conv_dw_b", "conv_ln_g", "conv_ln_b", "gla_norm",
           "ffn2_norm", "ple_norm", "ple_post_norm"]


def make_in_maps(inputs, n_cores, T):
    f = lambda a: np.ascontiguousarray(np.asarray(a, dtype=np.float32))
    shared = {}
    for k in _W_KEYS:
        shared[k] = f(inputs[k])[0]
    for k in _V_KEYS:
        shared[k] = f(inputs[k]).reshape(1, D)
    shared["final_norm"] = f(inputs["final_norm"]).reshape(1, D)
    shared["conv_dw_w"] = f(inputs["conv_dw_w"])[0]
    shared["gla_w_alpha"] = f(inputs["gla_w_alpha"])[0]
    shared["gla_b_alpha"] = f(inputs["gla_b_alpha"]).reshape(1, 512)
    x = f(inputs["x"])
    p = f(inputs["p"])[0]
    maps = []
    for c in range(n_cores):
        m = dict(shared)
        m["x"] = np.ascontiguousarray(x[c, :T])
        m["p"] = np.ascontiguousarray(p[c, :T])
        maps.append(m)
    return maps


def kernel(**inputs):
    B, T, _ = inputs["x"].shape
    nc = build_program(T)
    in_maps = make_in_maps(inputs, B, T)
    res = run_bass_kernel_spmd(nc, in_maps, core_ids=list(range(B)))
    out = np.stack([np.asarray(r["y"], dtype=np.float32) for r in res.results], axis=0)
    return out
```
